# Optimizing a Trainium2 kernel written in Bass

```python
import math
import jax
import jax.numpy as jnp
from jax import lax
import numpy as np

D_MODEL = 1024
BATCH = 8
SEQ = 2048
DEPTH = 1
DEC_BATCH = 128
DEC_SEQ = 8
PAST_LEN = 2048
PAGE_SIZE = 128

EPS = 1e-6
MEM_LEN = 256
N_BRANCH = 3
GLA_HEADS = 4
GLA_DK = 128
GLA_DV = 128
GLA_RANK = 16
GLA_TAU = 16.0
GLA_CHUNK = 64
FOX_HEADS = 8
FOX_HD = 64
FOX_QBLOCK = 128
FOX_FGATE_BIAS = 3.0
MEM_HEADS = 4
MEM_HD = 128
PEER_HEADS = 8
PEER_NKEYS = 128
PEER_NEXP = PEER_NKEYS * PEER_NKEYS
PEER_DKEY = 256
PEER_TOPK = 16
PEER_BLOCK = 128

GLA_W = GLA_HEADS * GLA_DK
GLA_VW = GLA_HEADS * GLA_DV
FOX_W = FOX_HEADS * FOX_HD
MEM_W = MEM_HEADS * MEM_HD
IN_WIDTHS = (GLA_W, GLA_W, GLA_VW, GLA_VW, GLA_RANK, FOX_W, FOX_W, FOX_W, FOX_HEADS, MEM_W, N_BRANCH * D_MODEL)
IN_W = sum(IN_WIDTHS)

kernel_name = 'hybrid_gla_fox_mem_peer_step'


def rmsnorm(x, g):
    xf = x.astype(jnp.float32)
    y = xf * lax.rsqrt(jnp.mean(xf * xf, axis=-1, keepdims=True) + EPS)
    return (y * g.astype(jnp.float32)).astype(x.dtype)


def project_in(n, lw):
    B, T, _ = n.shape
    z = n @ lw['w_in']
    parts = []
    off = 0
    for w in IN_WIDTHS:
        parts.append(z[..., off:off + w])
        off += w
    gq, gk, gv, gr, glr, fq, fk, fv, ff, mq, gt = parts
    q_gla = gq.reshape(B, T, GLA_HEADS, GLA_DK) * (GLA_DK ** -0.5)
    k_gla = gk.reshape(B, T, GLA_HEADS, GLA_DK)
    v_gla = gv.reshape(B, T, GLA_HEADS, GLA_DV)
    r_gla = jax.nn.silu(gr)
    la = jax.nn.log_sigmoid((glr @ lw['w_a2'] + lw['b_a2']).astype(jnp.float32)) / GLA_TAU
    la = la.reshape(B, T, GLA_HEADS, GLA_DK)
    q_fox = fq.reshape(B, T, FOX_HEADS, FOX_HD)
    k_fox = fk.reshape(B, T, FOX_HEADS, FOX_HD)
    v_fox = fv.reshape(B, T, FOX_HEADS, FOX_HD)
    logf = jax.nn.log_sigmoid((ff + lw['b_fgate']).astype(jnp.float32))
    q_mem = mq.reshape(B, T, MEM_HEADS, MEM_HD)
    gates = jax.nn.sigmoid(gt + lw['b_gate']).reshape(B, T, N_BRANCH, D_MODEL)
    return q_gla, k_gla, v_gla, r_gla, la, q_fox, k_fox, v_fox, logf, q_mem, gates


def gla_chunked(q, k, v, la, s0):
    B, T, H, dk = q.shape
    dv = v.shape[-1]
    c = math.gcd(T, GLA_CHUNK)
    nc = T // c

    def to_chunks(a):
        return a.reshape(B, nc, c, H, a.shape[-1]).transpose(1, 0, 3, 2, 4)

    mask = jnp.tril(jnp.ones((c, c), dtype=bool))[:, :, None]

    def step(s, inp):
        qc, kc, vc, lc = inp
        qf = qc.astype(jnp.float32)
        kf = kc.astype(jnp.float32)
        vf = vc.astype(jnp.float32)
        cum = jnp.cumsum(lc, axis=2)
        o_inter = jnp.einsum('bhtk,bhkv->bhtv', qf * jnp.exp(cum), s)
        diff = cum[:, :, :, None, :] - cum[:, :, None, :, :]
        decay = jnp.where(mask, jnp.exp(jnp.where(mask, diff, 0.0)), 0.0)
        att = jnp.einsum('bhtk,bhsk,bhtsk->bhts', qf, kf, decay)
        o_intra = jnp.einsum('bhts,bhsv->bhtv', att, vf)
        last = cum[:, :, -1:, :]
        s_new = jnp.exp(last[:, :, 0, :])[..., None] * s + jnp.einsum('bhsk,bhsv->bhkv', kf * jnp.exp(last - cum), vf)
        return s_new, o_inter + o_intra

    s_T, o = lax.scan(step, s0.astype(jnp.float32), (to_chunks(q), to_chunks(k), to_chunks(v), to_chunks(la)))
    o = o.transpose(1, 0, 3, 2, 4).reshape(B, T, H, dv)
    return o.astype(v.dtype), s_T.astype(s0.dtype)


def fox_attend(q, k, v, d_q, d_k, q_pos, k_pos):
    B, Tq, H, hd = q.shape
    qb = math.gcd(Tq, FOX_QBLOCK)
    nb = Tq // qb
    qs = q.reshape(B, nb, qb, H, hd).transpose(1, 0, 2, 3, 4)
    dqs = d_q.reshape(B, nb, qb, H).transpose(1, 0, 3, 2)
    ps = q_pos.reshape(nb, qb)
    dk_t = d_k.transpose(0, 2, 1)
    scale = FOX_HD ** -0.5

    def blk(args):
        qi, di, pi = args
        s = jnp.einsum('bqhd,bkhd->bhqk', qi, k).astype(jnp.float32) * scale
        s = s + (di[..., None] - dk_t[:, :, None, :])
        s = jnp.where(k_pos[None, :] <= pi[:, None], s, -jnp.inf)
        p = jax.nn.softmax(s, axis=-1)
        return jnp.einsum('bhqk,bkhd->bqhd', p.astype(v.dtype), v)

    o = lax.map(blk, (qs, dqs, ps))
    return o.transpose(1, 0, 2, 3, 4).reshape(B, Tq, H, hd)


def mem_kv(mem, g_mem, w_mem_kv):
    B, M, _ = mem.shape
    kv = rmsnorm(mem, g_mem) @ w_mem_kv
    mk = kv[..., :MEM_W].reshape(B, M, MEM_HEADS, MEM_HD)
    mv = kv[..., MEM_W:].reshape(B, M, MEM_HEADS, MEM_HD)
    return mk, mv


def mem_attend(q, mk, mv):
    s = jnp.einsum('bthd,bmhd->bhtm', q, mk).astype(jnp.float32) * (MEM_HD ** -0.5)
    p = jax.nn.softmax(s, axis=-1)
    return jnp.einsum('bhtm,bmhd->bthd', p.astype(mv.dtype), mv)


def merge_branches(o_gla, r_gla, o_fox, o_mem, gates, lw):
    B, T = o_gla.shape[:2]
    og = rmsnorm(o_gla, lw['g_gla_head']).reshape(B, T, GLA_VW) * r_gla
    m = (gates[:, :, 0] * (og @ lw['w_gla_o'])
         + gates[:, :, 1] * (o_fox.reshape(B, T, FOX_W) @ lw['w_fox_o'])
         + gates[:, :, 2] * (o_mem.reshape(B, T, MEM_W) @ lw['w_mem_o']))
    return m @ lw['w_out']


def peer_ffn(xn, w_pq, k1, k2, u_tab, v_tab):
    shp = xn.shape
    x2 = xn.reshape(-1, shp[-1])
    T = x2.shape[0]
    q = (x2 @ w_pq).reshape(T, PEER_HEADS, 2, PEER_DKEY // 2)
    s1 = jnp.einsum('thd,nd->thn', q[:, :, 0], k1).astype(jnp.float32)
    s2 = jnp.einsum('thd,nd->thn', q[:, :, 1], k2).astype(jnp.float32)
    v1, i1 = lax.top_k(s1, PEER_TOPK)
    v2, i2 = lax.top_k(s2, PEER_TOPK)
    cand = (v1[..., :, None] + v2[..., None, :]).reshape(T, PEER_HEADS, PEER_TOPK * PEER_TOPK)
    cidx = (i1[..., :, None] * PEER_NKEYS + i2[..., None, :]).reshape(T, PEER_HEADS, PEER_TOPK * PEER_TOPK)
    sc, pos = lax.top_k(cand, PEER_TOPK)
    eidx = jnp.take_along_axis(cidx, pos, axis=-1)
    g = jax.nn.softmax(sc, axis=-1)
    pad = (-T) % PEER_BLOCK
    nb = (T + pad) // PEER_BLOCK
    xp = jnp.pad(x2, ((0, pad), (0, 0))).reshape(nb, PEER_BLOCK, shp[-1])
    ip = jnp.pad(eidx, ((0, pad), (0, 0), (0, 0))).reshape(nb, PEER_BLOCK, PEER_HEADS, PEER_TOPK)
    gp = jnp.pad(g, ((0, pad), (0, 0), (0, 0))).reshape(nb, PEER_BLOCK, PEER_HEADS, PEER_TOPK)

    def blk(args):
        xb, ib, gb = args
        a = jax.nn.gelu(jnp.einsum('td,thkd->thk', xb, u_tab[ib]).astype(jnp.float32))
        return jnp.einsum('thk,thkd->td', (gb * a).astype(v_tab.dtype), v_tab[ib])

    out = lax.map(blk, (xp, ip, gp))
    return out.reshape(nb * PEER_BLOCK, shp[-1])[:T].reshape(shp).astype(xn.dtype)


def gather_pages(pool, page_table):
    g = pool[page_table]
    return g.reshape((page_table.shape[0], page_table.shape[1] * pool.shape[1]) + pool.shape[2:])


def hybrid_layer(x, mk, mv, gla_s0, past_k, past_v, past_logf, lw):
    B, T, _ = x.shape
    n = rmsnorm(x, lw['g_mix'])
    q_gla, k_gla, v_gla, r_gla, la, q_fox, k_fox, v_fox, logf, q_mem, gates = project_in(n, lw)
    o_gla, s_new = gla_chunked(q_gla, k_gla, v_gla, la, gla_s0)
    if past_k is None:
        k_all, v_all, lf_all = k_fox, v_fox, logf
        q_pos = jnp.arange(T)
    else:
        k_all = jnp.concatenate([past_k, k_fox], axis=1)
        v_all = jnp.concatenate([past_v, v_fox], axis=1)
        lf_all = jnp.concatenate([past_logf.astype(jnp.float32), logf], axis=1)
        q_pos = past_k.shape[1] + jnp.arange(T)
    k_pos = jnp.arange(k_all.shape[1])
    d_all = jnp.cumsum(lf_all, axis=1)
    o_fox = fox_attend(q_fox, k_all, v_all, d_all[:, -T:], d_all, q_pos, k_pos)
    o_mem = mem_attend(q_mem, mk, mv)
    h = x + merge_branches(o_gla, r_gla, o_fox, o_mem, gates, lw)
    h = h + peer_ffn(rmsnorm(h, lw['g_ffn']), lw['w_pq'], lw['peer_k1'], lw['peer_k2'], lw['peer_u'], lw['peer_v'])
    return h, k_fox, v_fox, logf, s_new


def setup_inputs(seed: int = 0) -> dict:
    key = jax.random.key(seed)
    k = jax.random.split(key, 30)
    n_pages = PAST_LEN // PAGE_SIZE
    n_used = DEC_BATCH * n_pages
    n_phys = n_used + n_used // 4

    def nrm(kk, shape, scale=1.0):
        return jax.random.normal(kk, shape, jnp.float32) * scale

    page_table = jax.random.permutation(k[8], n_phys)[:n_used].reshape(DEC_BATCH, n_pages).astype(jnp.int32)
    return {
        'x_prompt': nrm(k[0], (BATCH, SEQ, D_MODEL)),
        'x_sample': nrm(k[1], (DEC_BATCH, DEC_SEQ, D_MODEL)),
        'cache_fox_k': nrm(k[2], (DEPTH, n_phys, PAGE_SIZE, FOX_HEADS, FOX_HD)),
        'cache_fox_v': nrm(k[3], (DEPTH, n_phys, PAGE_SIZE, FOX_HEADS, FOX_HD)),
        'cache_fox_logf': jax.nn.log_sigmoid(nrm(k[4], (DEPTH, n_phys, PAGE_SIZE, FOX_HEADS)) + FOX_FGATE_BIAS),
        'state_gla': nrm(k[5], (DEPTH, DEC_BATCH, GLA_HEADS, GLA_DK, GLA_DV)),
        'cache_mem_k': nrm(k[6], (DEPTH, DEC_BATCH, MEM_LEN, MEM_HEADS, MEM_HD)),
        'cache_mem_v': nrm(k[7], (DEPTH, DEC_BATCH, MEM_LEN, MEM_HEADS, MEM_HD)),
        'page_table': page_table,
        'mem_prompt': nrm(k[9], (BATCH, MEM_LEN, D_MODEL)),
        'g_mix': 1.0 + nrm(k[10], (DEPTH, D_MODEL), 0.02),
        'w_in': nrm(k[11], (DEPTH, D_MODEL, IN_W), D_MODEL ** -0.5),
        'w_a2': nrm(k[12], (DEPTH, GLA_RANK, GLA_W), GLA_RANK ** -0.5),
        'b_a2': nrm(k[13], (DEPTH, GLA_W), 0.1),
        'b_fgate': FOX_FGATE_BIAS + nrm(k[14], (DEPTH, FOX_HEADS), 0.1),
        'b_gate': nrm(k[15], (DEPTH, N_BRANCH * D_MODEL), 0.02),
        'g_gla_head': 1.0 + nrm(k[16], (DEPTH, GLA_DV), 0.02),
        'w_gla_o': nrm(k[17], (DEPTH, GLA_VW, D_MODEL), GLA_VW ** -0.5),
        'w_fox_o': nrm(k[18], (DEPTH, FOX_W, D_MODEL), FOX_W ** -0.5),
        'w_mem_o': nrm(k[19], (DEPTH, MEM_W, D_MODEL), MEM_W ** -0.5),
        'w_out': nrm(k[20], (DEPTH, D_MODEL, D_MODEL), D_MODEL ** -0.5),
        'g_mem': 1.0 + nrm(k[21], (DEPTH, D_MODEL), 0.02),
        'w_mem_kv': nrm(k[22], (DEPTH, D_MODEL, 2 * MEM_W), D_MODEL ** -0.5),
        'g_ffn': 1.0 + nrm(k[23], (DEPTH, D_MODEL), 0.02),
        'w_pq': nrm(k[24], (DEPTH, D_MODEL, PEER_HEADS * PEER_DKEY), D_MODEL ** -0.5),
        'peer_k1': nrm(k[25], (DEPTH, PEER_NKEYS, PEER_DKEY // 2), (PEER_DKEY // 2) ** -0.5),
        'peer_k2': nrm(k[26], (DEPTH, PEER_NKEYS, PEER_DKEY // 2), (PEER_DKEY // 2) ** -0.5),
        'peer_u': nrm(k[27], (DEPTH, PEER_NEXP, D_MODEL), D_MODEL ** -0.5),
        'peer_v': nrm(k[28], (DEPTH, PEER_NEXP, D_MODEL), PEER_HEADS ** -0.5),
        'g_final': 1.0 + nrm(k[29], (D_MODEL,), 0.02),
    }


def reference(x_prompt, x_sample, cache_fox_k, cache_fox_v, cache_fox_logf, state_gla, cache_mem_k, cache_mem_v,
              page_table, mem_prompt, g_mix, w_in, w_a2, b_a2, b_fgate, b_gate, g_gla_head, w_gla_o, w_fox_o,
              w_mem_o, w_out, g_mem, w_mem_kv, g_ffn, w_pq, peer_k1, peer_k2, peer_u, peer_v, g_final):
    hp = x_prompt
    hs = x_sample
    kp_l, vp_l, lfp_l, sp_l, mkp_l, mvp_l = [], [], [], [], [], []
    ks_l, vs_l, lfs_l, ss_l = [], [], [], []
    for l in range(DEPTH):
        lw = {
            'g_mix': g_mix[l], 'w_in': w_in[l], 'w_a2': w_a2[l], 'b_a2': b_a2[l], 'b_fgate': b_fgate[l],
            'b_gate': b_gate[l], 'g_gla_head': g_gla_head[l], 'w_gla_o': w_gla_o[l], 'w_fox_o': w_fox_o[l],
            'w_mem_o': w_mem_o[l], 'w_out': w_out[l], 'g_ffn': g_ffn[l], 'w_pq': w_pq[l],
            'peer_k1': peer_k1[l], 'peer_k2': peer_k2[l], 'peer_u': peer_u[l], 'peer_v': peer_v[l],
        }
        mk_p, mv_p = mem_kv(mem_prompt, g_mem[l], w_mem_kv[l])
        s0_p = jnp.zeros((hp.shape[0], GLA_HEADS, GLA_DK, GLA_DV), hp.dtype)
        hp, kp, vp, lfp, sp = hybrid_layer(hp, mk_p, mv_p, s0_p, None, None, None, lw)
        pk = gather_pages(cache_fox_k[l], page_table)
        pv = gather_pages(cache_fox_v[l], page_table)
        plf = gather_pages(cache_fox_logf[l], page_table)
        hs, ks, vs, lfs, ss = hybrid_layer(hs, cache_mem_k[l], cache_mem_v[l], state_gla[l], pk, pv, plf, lw)
        kp_l.append(kp); vp_l.append(vp); lfp_l.append(lfp); sp_l.append(sp); mkp_l.append(mk_p); mvp_l.append(mv_p)
        ks_l.append(ks); vs_l.append(vs); lfs_l.append(lfs); ss_l.append(ss)
    y_prompt = rmsnorm(hp, g_final)
    y_sample = rmsnorm(hs, g_final)
    fox_k_prompt = jnp.stack(kp_l)
    fox_v_prompt = jnp.stack(vp_l)
    fox_logf_prompt = jnp.stack(lfp_l)
    gla_state_prompt = jnp.stack(sp_l)
    mem_k_prompt = jnp.stack(mkp_l)
    mem_v_prompt = jnp.stack(mvp_l)
    fox_k_sample = jnp.stack(ks_l)
    fox_v_sample = jnp.stack(vs_l)
    fox_logf_sample = jnp.stack(lfs_l)
    gla_state_sample = jnp.stack(ss_l)
    return (y_prompt, y_sample, fox_k_prompt, fox_v_prompt, fox_logf_prompt, gla_state_prompt, mem_k_prompt,
            mem_v_prompt, fox_k_sample, fox_v_sample, fox_logf_sample, gla_state_sample)
```

```python
from contextlib import ExitStack
import numpy as np
import concourse.bass as bass
import concourse.mybir as mybir
from concourse.bass_utils import run_bass_kernel_spmd

F32 = mybir.dt.float32
BF16 = mybir.dt.bfloat16
U32 = mybir.dt.uint32
I32 = mybir.dt.int32
AF = mybir.ActivationFunctionType
ALU = mybir.AluOpType
AX = mybir.AxisListType

PE, ACT, DVE, POOL, SP = "tensor", "scalar", "vector", "gpsimd", "sync"
ENGS = [PE, ACT, DVE, POOL, SP]

NCORES = 8
D = 1024
SEQ = 2048
NT = SEQ // 128
EPS = 1e-6
IN_W = 7192
C_GQ, C_GK, C_GV, C_GR, C_GLR, C_FQ, C_FK, C_FV, C_FF, C_MQ, C_GT = (
    0, 512, 1024, 1536, 2048, 2064, 2576, 3088, 3600, 3608, 4120)
NPHYS = 2560
NEG = -1.0e30


class Op:
    __slots__ = ("eng", "fn", "reads", "writes", "dma", "deps", "sig", "tok", "idx", "grp", "slot", "clear")

    def __init__(self, eng, fn, reads, writes, dma, grp=None):
        self.eng = eng
        self.fn = fn
        self.reads = reads
        self.writes = writes
        self.dma = dma
        self.deps = []
        self.sig = False
        self.tok = None
        self.grp = grp
        self.slot = None


class Prog:
    N_DMA_SLOTS = 96

    def __init__(self, nc):
        self.nc = nc
        self.ops = []

    EXPAND = {"bank0": ("bank0h0", "bank0h1")}

    def _x(self, names):
        out = []
        for n in names:
            out.extend(self.EXPAND.get(n, (n,)))
        return tuple(out)

    def op(self, eng, fn, reads=(), writes=()):
        reads = self._x(reads); writes = self._x(writes)
        writes = tuple(writes) + tuple(r for r in reads if r.startswith("bank") and r not in writes)
        o = Op(eng, fn, reads, writes, False)
        self.ops.append(o)
        return o

    def dma(self, eng, fn, reads=(), writes=(), grp=None):
        o = Op(eng, fn, self._x(reads), self._x(writes), True, grp)
        self.ops.append(o)
        return o

    def finalize(self, es):
        nc = self.nc
        last_w = {}
        readers = {}
        for i, o in enumerate(self.ops):
            o.idx = i
            deps = set()
            for r in o.reads:
                w = last_w.get(r)
                if w is not None:
                    deps.add(w)
            for r in o.writes:
                w = last_w.get(r)
                if w is not None:
                    deps.add(w)
                for rd in readers.get(r, ()):
                    deps.add(rd)
            deps.discard(o)
            for r in o.reads:
                readers.setdefault(r, []).append(o)
            for r in o.writes:
                last_w[r] = o
                readers[r] = []
            dl = []
            for d in deps:
                if d.eng == PE and o.eng == PE and not d.dma and not o.dma:
                    continue
                d.sig = True
                dl.append(d)
            o.deps = dl
        esem = {e: es.enter_context(nc.semaphore("s_" + e)) for e in ENGS}
        NS = self.N_DMA_SLOTS
        NH = 24
        slots = [es.enter_context(nc.semaphore("d%d" % i)) for i in range(NS)]
        consumers = {}
        for o in self.ops:
            for d in o.deps:
                if d.dma:
                    consumers.setdefault(d, []).append(o)
        slot_val = [0] * NS
        slot_last = [None] * NS
        nh = nsw = 0
        for o in self.ops:
            if not o.dma:
                continue
            o.clear = False
            if o.eng == POOL:
                s = NH + nsw % (NS - NH)
                nsw += 1
                p = slot_last[s]
                if p is not None:
                    o.deps.append(p)
                slot_val[s] += 16
            else:
                s = nh % NH
                nh += 1
                p = slot_last[s]
                if p is not None:
                    o.deps.append(p)
                slot_val[s] += 16
            o.slot = s
            o.tok = (s, slot_val[s])
            slot_last[s] = o
        cnt = {e: 0 for e in ENGS}
        for o in self.ops:
            if not o.dma and o.sig:
                cnt[o.eng] += 1
                o.tok = (esem[o.eng], cnt[o.eng])
        self.max_sem = max(list(cnt.values()) + slot_val)

        def tok_of(d):
            if d.dma:
                return slots[d.tok[0]], d.tok[1]
            return d.tok

        per_eng = {e: [] for e in ENGS}
        for o in self.ops:
            per_eng[o.eng].append(o)

        def emit_all(e, ename):
            known = {}
            seen_sw = set()
            for o in per_eng[ename]:
                for d in o.deps:
                    sem, val = tok_of(d)
                    k = sem.num
                    if known.get(k, 0) < val:
                        e.wait_ge(sem, val)
                        known[k] = val
                if o.fn is None:
                    continue
                ins = o.fn(e)
                if o.dma:
                    ins.then_inc(slots[o.slot], 16)
                elif o.sig:
                    ins.then_inc(esem[ename], 1)

        block = es.enter_context(nc.Block())

        @block.tensor
        def _(e):
            emit_all(e, PE)

        @block.scalar
        def _(e):
            emit_all(e, ACT)

        @block.vector
        def _(e):
            emit_all(e, DVE)

        @block.gpsimd
        def _(e):
            emit_all(e, POOL)

        @block.sync
        def _(e):
            emit_all(e, SP)


def bc_rows(dram_ap_row, n):
    t = dram_ap_row
    return bass.AP(t.tensor, t.offset, [[0, 128], [1, n]])


def build(stage=99, nphys=NPHYS, ntiles=NT, sample=1, maxops=None, nexp=16384):
    nc = bass.Bass("TRN2", target_bir_lowering=False)

    def din(name, shape, dt=F32):
        return nc.dram_tensor(name, list(shape), dt, kind="ExternalInput").ap()

    def dout(name, shape, dt=F32):
        return nc.dram_tensor(name, list(shape), dt, kind="ExternalOutput").ap()

    xp = din("xp", [SEQ, D]); xs = din("xs", [128, D]); memp = din("memp", [256, D])
    ck = din("ck", [nphys * 128, 512]); cv = din("cv", [nphys * 128, 512]); clf = din("clf", [nphys * 128, 8])
    sg = din("sg", [64, 128, 128]); cmk = din("cmk", [16, 256, 512]); cmv = din("cmv", [16, 256, 512])
    pt = din("pt", [1, 256], I32)
    g_mix = din("g_mix", [1, D]); w_in = din("w_in", [D, IN_W]); w_a2 = din("w_a2", [16, 512])
    b_a2 = din("b_a2", [1, 512]); b_fgate = din("b_fgate", [1, 8]); b_gate = din("b_gate", [1, 3072])
    g_gh = din("g_gh", [1, 128]); w_gla_o = din("w_gla_o", [512, D]); w_fox_o = din("w_fox_o", [512, D])
    w_mem_o = din("w_mem_o", [512, D]); w_out = din("w_out", [D, D]); g_mem = din("g_mem", [1, D])
    w_mem_kv = din("w_mem_kv", [D, D]); g_ffn = din("g_ffn", [1, D]); w_pq = din("w_pq", [D, 2048])
    pk1 = din("pk1", [128, 128]); pk2 = din("pk2", [128, 128])
    pu = din("pu", [nexp, D]); pv = din("pv", [nexp, D]); g_final = din("g_final", [1, D])
    cst = din("cst", [128, 560])
    cst2 = din("cst2", [128, 760])

    yp = dout("yp", [SEQ, D]); ys = dout("ys", [128, D])
    fkp = dout("fkp", [SEQ, 512]); fvp = dout("fvp", [SEQ, 512]); lfp = dout("lfp", [SEQ, 8])
    gsp = dout("gsp", [4, 128, 128]); mkp = dout("mkp", [256, 512]); mvp = dout("mvp", [256, 512])
    fks = dout("fks", [128, 512]); fvs = dout("fvs", [128, 512]); lfs = dout("lfs", [128, 8])
    gss = dout("gss", [64, 128, 128])

    es = ExitStack()
    P = Prog(nc)
    uid = [0]

    sb_log = []

    def sb(shape, dt=F32, name=None):
        uid[0] += 1
        n = 1
        for d in shape[1:]:
            n *= d
        sb_log.append((name, n * (2 if dt == BF16 else 4)))
        try:
            return es.enter_context(nc.sbuf_tensor(name or ("t%d" % uid[0]), list(shape), dt))
        except AssertionError:
            print("SBUF:", sum(b for _, b in sb_log), sorted(sb_log, key=lambda x: -x[1])[:60])
            raise

    def pst(shape, dt=F32, name=None):
        uid[0] += 1
        return es.enter_context(nc.psum_tensor(name or ("p%d" % uid[0]), list(shape), dt))

    banks = [pst([128, 512], F32, "bank%d" % i) for i in range(8)]

    def bk(i):
        return banks[i]

    def bk_bf(i):
        return banks[i][:].bitcast(BF16)

    cst_f = sb([128, 560], F32, "cst_f")
    P.dma(SP, lambda e: e.dma_start(out=cst_f[:], in_=cst[:, :]), [], ["cst_f"])
    cst2_f = sb([128, 760], F32, "cst2_f")
    P.dma(SP, lambda e: e.dma_start(out=cst2_f[:], in_=cst2[:, :]), [], ["cst2_f"])
    Mst_f = cst2_f[:, 0:128]
    Bsel_f = [cst2_f[:, 128:256], cst2_f[:, 256:384]]
    bustr8_f = cst2_f[:, 384:512]
    Esel_f = cst2_f[:, 512:760]
    bmask = cst_f[:, 528:544]
    ident_f = cst_f[:, 0:128]
    tri_f = cst_f[:, 128:256]
    btri_f = cst_f[:, 256:384]
    ustr_f = cst_f[:, 384:512]
    iota16_f = cst_f[:, 512:528]
    ident_b = sb([128, 128], BF16, "ident_b")
    ones_b = sb([128, 128], BF16, "ones_b")
    ones_f = sb([128, 128], F32, "ones_f")
    tri_b = sb([128, 128], BF16, "tri_b")
    btri_b = sb([128, 128], BF16, "btri_b")
    ntri16 = sb([128, 128], F32, "ntri16")
    nbtri16 = sb([128, 128], F32, "nbtri16")
    P.op(DVE, lambda e: e.tensor_copy(out=ident_b[:], in_=ident_f), ["cst_f"], ["ident_b"])
    P.op(DVE, lambda e: e.memset(ones_b[:], 1.0), [], ["ones_b"])
    P.op(DVE, lambda e: e.memset(ones_f[:], 1.0), [], ["ones_f"])
    P.op(DVE, lambda e: e.tensor_copy(out=tri_b[:], in_=tri_f), ["cst_f"], ["tri_b"])
    P.op(DVE, lambda e: e.tensor_copy(out=btri_b[:], in_=btri_f), ["cst_f"], ["btri_b"])
    P.op(DVE, lambda e: e.tensor_scalar(out=ntri16[:], in0=tri_f, scalar1=-1.0 / 16.0, scalar2=None, op0=ALU.mult),
         ["cst_f"], ["ntri16"])
    P.op(DVE, lambda e: e.tensor_scalar(out=nbtri16[:], in0=btri_f, scalar1=-1.0 / 16.0, scalar2=None, op0=ALU.mult),
         ["cst_f"], ["nbtri16"])

    gmix_bc = sb([128, D], F32, "gmix_bc"); gffn_bc = sb([128, D], F32, "gffn_bc")
    gfin_bc = sb([128, D], F32, "gfin_bc"); acc = sb([128, D], F32, "acc")
    gmem_bc = acc
    for tl, src, nm in ((gmix_bc, g_mix, "gmix_bc"), (gffn_bc, g_ffn, "gffn_bc"), (gfin_bc, g_final, "gfin_bc"),
                        (gmem_bc, g_mem, "acc")):
        P.dma(SP, (lambda tl, src: lambda e: e.dma_start(out=tl[:], in_=bc_rows(src, D)))(tl, src), [], [nm])
    bfg_bc = sb([128, 8], F32, "bfg_bc")
    P.dma(SP, lambda e: e.dma_start(out=bfg_bc[:], in_=bc_rows(b_fgate, 8)), [], ["bfg_bc"])
    bg_all = sb([128, 25], F32, "bgate_c")
    bgate_c = bg_all[:, 0:24]
    ggh_c = bg_all[:, 24:25]
    stg = sb([25, 128], F32, "stg")
    P.dma(SP, lambda e: e.dma_start(out=stg[0:24, :], in_=b_gate.rearrange("o (c p) -> (o c) p", p=128)), [], ["stg"])
    P.dma(SP, lambda e: e.dma_start(out=stg[24:25, :], in_=g_gh[:, :]), [], ["stg"])
    P.op(PE, lambda e: e.transpose(out=banks[0][:, 0:25], in_=stg[0:25, :], identity=ident_f[0:25, 0:25]),
         ["stg", "cst_f"], ["bank0"])
    P.op(ACT, lambda e: e.activation(out=bg_all[:], in_=banks[0][:, 0:25], func=AF.Copy), ["bank0"], ["bgate_c"])
    wa2_f = sb([17, 512], F32, "wa2_f")
    P.dma(SP, lambda e: e.dma_start(out=wa2_f[0:16, :], in_=w_a2[:, :]), [], ["wa2_f"])
    P.dma(SP, lambda e: e.dma_start(out=wa2_f[16:17, :], in_=b_a2[:, :]), [], ["wa2_f"])

    NRING = 2
    ring = [sb([128, 4096], BF16, "ring%d" % i) for i in range(NRING)]
    ring_i = [0]

    def wload(dram_view, shape3):
        i = ring_i[0] % NRING
        ring_i[0] += 1
        a, b = shape3
        t = ring[i]
        dst = t[:, 0:a * b].rearrange("p (a b) -> p a b", a=a)
        nm = "ring%d" % i
        P.dma(POOL, lambda e: e.dma_start(out=dst, in_=dram_view), [], [nm])
        return dst, nm

    w_in_v = w_in.rearrange("(c p) n -> p c n", p=128)

    PS = 512
    x_t = sb([128, D], F32, "x_t"); junk_b = sb([128, D], BF16, "junk_b")
    ss = sb([128, 1], F32, "ss"); rstd = sb([128, 1], F32, "rstd")
    n_b = sb([128, D], BF16, "n_b"); nT = sb([128, 8, 128], BF16, "nT")
    fk_tm = sb([128, 512], F32, "fk_tm"); fv_tm = sb([128, 512], F32, "fv_tm")
    KT = sb([128, 4, SEQ], BF16, "KT")
    VaugR = sb([128, NT * 8 * 66], BF16, "VaugR")
    Vaug = VaugR[:].rearrange("p (a h d) -> p a h d", a=NT, h=8)
    VAUG_ALL = ["Vaug%d" % i for i in range(NT)]
    lf_t = sb([128, 8], F32, "lf_t"); lf_e = sb([128, 8], F32, "lf_e")
    qT_f = sb([128, 4, 128], F32, "qT_f"); kT_f = sb([128, 4, 128], F32, "kT_f")
    v_b = sb([128, 512], BF16, "v_b"); rT = sb([128, 4, 128], BF16, "rT")
    glrT = sb([17, 128], F32, "glrT")
    P.op(DVE, lambda e: e.memset(glrT[:], 1.0), [], ["glrT"])
    qfT = sb([128, 4, 128], BF16, "qfT"); qmT = sb([128, 4, 128], BF16, "qmT")
    gatesT = sb([128, 24, 128], BF16, "gatesT")
    for q in range(4):
        P.op(DVE, (lambda q: lambda e: e.memset(VaugR[:, q * 2112:(q + 1) * 2112], 1.0))(q), [], VAUG_ALL)

    def pstride(t):
        return t[:].ap[0][0]

    def AP(t, off, dims):
        return bass.AP(t[:].tensor, off, [[pstride(t), 128]] + [list(d) for d in dims])

    bank_rr = [0]

    def nbank():
        b = 1 + bank_rr[0] % 3
        bank_rr[0] += 1
        return b

    def rmsnorm_rows(src, src_res, g_bc, g_res, dst, dst_res):
        P.op(ACT, lambda e: e.activation(out=junk_b[:], in_=src, func=AF.Square, accum_out=ss[:]),
             [src_res], ["junk_b", "ss"])
        P.op(DVE, lambda e: e.tensor_scalar(out=rstd[:], in0=ss[:], scalar1=1.0 / D, scalar2=EPS,
                                            op0=ALU.mult, op1=ALU.add), ["ss"], ["rstd"])
        P.op(ACT, lambda e: e.activation(out=rstd[:], in_=rstd[:], func=AF.Ln), ["rstd"], ["rstd"])
        P.op(ACT, lambda e: e.activation(out=rstd[:], in_=rstd[:], func=AF.Exp, scale=-0.5), ["rstd"], ["rstd"])
        P.op(DVE, lambda e: e.scalar_tensor_tensor(out=dst, in0=src, scalar=rstd[:], in1=g_bc[:],
                                                   op0=ALU.mult, op1=ALU.mult),
             [src_res, "rstd", g_res], [dst_res])

    def transpose_rows(src_b, src_res, dstT, dst_res, nchunk=8):
        pb = bk_bf(0)
        for c in range(nchunk):
            P.op(PE, (lambda c: lambda e: e.transpose(out=pb[:, c * 128:(c + 1) * 128],
                                                       in_=src_b[:, c * 128:(c + 1) * 128], identity=ident_b[:]))(c),
                 [src_res, "ident_b"], ["bank0"])
        P.op(ACT, lambda e: e.activation(out=dstT[:].rearrange("p c t -> p (c t)"), in_=pb[:, 0:nchunk * 128],
                                         func=AF.Copy), ["bank0"], [dst_res])

    def proj_tm(wview, ncols, evac, src=None, src_res="nT"):
        src = nT if src is None else src
        wv, wn = wload(wview, (8, ncols))
        b = nbank()
        pb = bk(b)
        for c in range(8):
            P.op(PE, (lambda c: lambda e: e.matmul(pb[:, 0:ncols], lhsT=src[:, c, :], rhs=wv[:, c, :],
                                                    start=(c == 0), stop=(c == 7)))(c),
                 [src_res, wn], ["bank%d" % b])
        evac(pb[:, 0:ncols], "bank%d" % b)

    def proj_fm(wview, ncols, evac, src=None, src_res="nT"):
        src = nT if src is None else src
        wv, wn = wload(wview, (8, ncols))
        b = nbank()
        pb = bk(b)
        nch = (ncols + 127) // 128
        for j in range(nch):
            w = min(128, ncols - j * 128)
            for c in range(8):
                P.op(PE, (lambda c, j, w: lambda e: e.matmul(pb[0:w, j * 128:(j + 1) * 128],
                                                             lhsT=wv[:, c, j * 128:j * 128 + w], rhs=src[:, c, :],
                                                             start=(c == 0), stop=(c == 7)))(c, j, w),
                     [src_res, wn], ["bank%d" % b])
        evac(pb[:, 0:512], "bank%d" % b)

    def win(c0, n):
        return w_in_v[:, :, c0:c0 + n]

    out_res = []

    def ores():
        nm = "out%d" % len(out_res)
        out_res.append(nm)
        return nm

    KTn = sb([128, 4, 128], BF16, "KTn"); Vn = sb([128, 8, 66], BF16, "Vn")
    P.op(DVE, lambda e: e.memset(Vn[:].rearrange("p h d -> p (h d)"), 1.0), [], ["Vn"])

    def front(x_src, ti, is_sample):
        P.dma(SP, lambda e: e.dma_start(out=x_t[:], in_=x_src), [], ["x_t"])
        rmsnorm_rows(x_t[:], "x_t", gmix_bc, "gmix_bc", n_b[:], "n_b")
        transpose_rows(n_b, "n_b", nT, "nT")
        fk_out = (fks[:, :] if is_sample else fkp[ti * 128:(ti + 1) * 128, :])
        fv_out = (fvs[:, :] if is_sample else fvp[ti * 128:(ti + 1) * 128, :])
        lf_out = (lfs[:, :] if is_sample else lfp[ti * 128:(ti + 1) * 128, :])

        def ev_fk(ps, res):
            P.op(ACT, lambda e: e.activation(out=fk_tm[:], in_=ps, func=AF.Copy), [res], ["fk_tm"])
            P.dma(SP, lambda e: e.dma_start(out=fk_out, in_=fk_tm[:]), ["fk_tm"], [ores()])
        proj_tm(win(C_FK, 512), 512, ev_fk)

        def ev_fv(ps, res):
            P.op(ACT, lambda e: e.activation(out=fv_tm[:], in_=ps, func=AF.Copy), [res], ["fv_tm"])
            if is_sample:
                P.op(DVE, lambda e: e.tensor_copy(out=Vn[:, :, 0:64],
                                                  in_=fv_tm[:].rearrange("p (h d) -> p h d", h=8)), ["fv_tm"], ["Vn"])
            else:
                P.op(DVE, lambda e: e.tensor_copy(out=Vaug[:, ti, :, 0:64],
                                                  in_=fv_tm[:].rearrange("p (h d) -> p h d", h=8)), ["fv_tm"], ["Vaug%d" % ti])
            P.dma(SP, lambda e: e.dma_start(out=fv_out, in_=fv_tm[:]), ["fv_tm"], [ores()])
        proj_tm(win(C_FV, 512), 512, ev_fv)

        def ev_ff(ps, res):
            P.op(DVE, lambda e: e.tensor_tensor(out=lf_e[:], in0=ps, in1=bfg_bc[:], op=ALU.add),
                 [res, "bfg_bc"], ["lf_e"])
            P.op(ACT, lambda e: e.activation(out=lf_e[:], in_=lf_e[:], func=AF.Exp, scale=-1.0), ["lf_e"], ["lf_e"])
            P.op(ACT, lambda e: e.activation(out=lf_e[:], in_=lf_e[:], func=AF.Ln, bias=1.0), ["lf_e"], ["lf_e"])
            P.op(DVE, lambda e: e.tensor_scalar(out=lf_t[:], in0=lf_e[:], scalar1=-1.0, scalar2=None, op0=ALU.mult),
                 ["lf_e"], ["lf_t"])
            P.dma(SP, lambda e: e.dma_start(out=lf_out, in_=lf_t[:]), ["lf_t"], [ores()])
        proj_tm(win(C_FF, 8), 8, ev_ff)

        def cp(dst, res_dst, func=AF.Copy):
            def ev(ps, res):
                P.op(ACT, lambda e: e.activation(out=dst, in_=ps, func=func), [res], [res_dst])
            return ev
        proj_fm(win(C_GQ, 512), 512, cp(qT_f[:].rearrange("p c t -> p (c t)"), "qT_f"))
        proj_fm(win(C_GK, 512), 512, cp(kT_f[:].rearrange("p c t -> p (c t)"), "kT_f"))
        proj_tm(win(C_GV, 512), 512, cp(v_b[:], "v_b"))
        proj_fm(win(C_GR, 512), 512, cp(rT[:].rearrange("p c t -> p (c t)"), "rT", AF.Silu))

        def ev_glr(ps, res):
            P.op(ACT, lambda e: e.activation(out=glrT[0:16, :], in_=ps[0:16, 0:128], func=AF.Copy), [res], ["glrT"])
        proj_fm(win(C_GLR, 16), 16, ev_glr)
        proj_fm(win(C_FQ, 512), 512, cp(qfT[:].rearrange("p c t -> p (c t)"), "qfT"))

        def ev_kT(ps, res):
            if is_sample:
                P.op(ACT, lambda e: e.activation(out=KTn[:], in_=ps.rearrange("p (c t) -> p c t", c=4), func=AF.Copy),
                     [res], ["KTn"])
            else:
                P.op(ACT, lambda e: e.activation(out=KT[:, :, ti * 128:(ti + 1) * 128],
                                                 in_=ps.rearrange("p (c t) -> p c t", c=4), func=AF.Copy),
                     [res], ["KT%d" % ti])
        proj_fm(win(C_FK, 512), 512, ev_kT)
        proj_fm(win(C_MQ, 512), 512, cp(qmT[:].rearrange("p c t -> p (c t)"), "qmT"))
        for gb in range(6):
            def ev_g(ps, res, gb=gb):
                for j in range(4):
                    ch = gb * 4 + j
                    P.op(ACT, (lambda j, ch: lambda e: e.activation(out=gatesT[:, ch, :],
                                                                    in_=ps[:, j * 128:(j + 1) * 128],
                                                                    func=AF.Sigmoid, bias=bg_all[:, ch:ch + 1]))(j, ch),
                         [res, "bgate_c"], ["gatesT%d" % ch])
            proj_fm(win(C_GT + gb * 512, 512), 512, ev_g)

    sp_f = sb([128, 512], F32, "sp_f")
    Epos = sb([128, 4, 128], F32, "Epos"); Eneg = sb([128, 4, 128], F32, "Eneg")
    qtl = sb([128, 4, 128], BF16, "qtl"); ktl = sb([128, 4, 128], BF16, "ktl"); khT = sb([128, 4, 128], BF16, "khT")
    kh = sb([128, 512], BF16, "kh"); attT = sb([128, 4, 128], BF16, "attT")
    S_f = sb([128, 4, 128], F32, "S_f"); S_b = sb([128, 4, 128], BF16, "S_b")
    o_sb = sb([128, 512], F32, "o_sb"); sq_b = sb([128, 512], BF16, "sq_b")
    sd_f = sb([128, 512], F32, "sd_f"); rs_f = sd_f; t1_f = o_sb
    ogT = sb([128, 4, 128], BF16, "ogT")
    vm_b = [sb([128, 512], BF16, "vm_b%d" % i) for i in range(2)]
    P.op(DVE, lambda e: e.memset(S_f[:].rearrange("p h v -> p (h v)"), 0.0), [], ["S_f"])
    P.op(DVE, lambda e: e.memset(S_b[:].rearrange("p h v -> p (h v)"), 0.0), [], ["S_b"])

    def gla(L, nmask16, nmask_res, mask_b, mask_res, seq_states):
        nseq = 128 // L
        b1 = nbank()
        P.op(PE, lambda e: e.matmul(bk(b1)[:, :], lhsT=glrT[:, :], rhs=wa2_f[:, :], start=True, stop=True),
             ["glrT", "wa2_f"], ["bank%d" % b1])
        P.op(ACT, lambda e: e.activation(out=sp_f[:], in_=bk(b1)[:, :], func=AF.Exp, scale=-1.0),
             ["bank%d" % b1], ["sp_f"])
        P.op(ACT, lambda e: e.activation(out=sp_f[:], in_=sp_f[:], func=AF.Ln, bias=1.0), ["sp_f"], ["sp_f"])
        b2 = nbank()
        for h in range(4):
            P.op(PE, (lambda h: lambda e: e.matmul(bk(b2)[:, h * 128:(h + 1) * 128], lhsT=sp_f[:, h * 128:(h + 1) * 128],
                                                    rhs=nmask16[:], start=True, stop=True))(h),
                 ["sp_f", nmask_res], ["bank%d" % b2])
        fl = lambda t: t[:].rearrange("p c t -> p (c t)")
        P.op(ACT, lambda e: e.activation(out=fl(Epos), in_=bk(b2)[:, :], func=AF.Exp), ["bank%d" % b2], ["Epos"])
        P.op(ACT, lambda e: e.activation(out=fl(Eneg), in_=bk(b2)[:, :], func=AF.Exp, scale=-1.0),
             ["bank%d" % b2], ["Eneg"])
        P.op(DVE, lambda e: e.scalar_tensor_tensor(out=fl(qtl), in0=fl(qT_f), scalar=128.0 ** -0.5, in1=fl(Epos),
                                                   op0=ALU.mult, op1=ALU.mult), ["qT_f", "Epos"], ["qtl"])
        P.op(DVE, lambda e: e.tensor_tensor(out=fl(ktl), in0=fl(kT_f), in1=fl(Eneg), op=ALU.mult),
             ["kT_f", "Eneg"], ["ktl"])
        P.op(DVE, lambda e: e.tensor_tensor(
            out=AP(khT, 0, [[128, 4], [L, nseq], [1, L]]), in0=AP(ktl, 0, [[128, 4], [L, nseq], [1, L]]),
            in1=AP(Epos, L - 1, [[128, 4], [L, nseq], [0, L]]), op=ALU.mult), ["ktl", "Epos"], ["khT"])
        pb0 = bk_bf(0)
        for h in range(4):
            P.op(PE, (lambda h: lambda e: e.transpose(out=pb0[:, h * 128:(h + 1) * 128], in_=khT[:, h, :],
                                                       identity=ident_b[:]))(h), ["khT", "ident_b"], ["bank0"])
        P.op(ACT, lambda e: e.activation(out=kh[:], in_=pb0[:, 0:512], func=AF.Copy), ["bank0"], ["kh"])
        b3 = nbank()
        for h in range(4):
            P.op(PE, (lambda h: lambda e: e.matmul(bk(b3)[:, h * 128:(h + 1) * 128], lhsT=ktl[:, h, :], rhs=qtl[:, h, :],
                                                    start=True, stop=True))(h), ["ktl", "qtl"], ["bank%d" % b3])
        P.op(DVE, lambda e: e.tensor_tensor(out=attT[:], in0=bk(b3)[:, :].rearrange("p (h t) -> p h t", h=4),
                                            in1=AP(mask_b, 0, [[0, 4], [1, 128]]), op=ALU.mult),
             ["bank%d" % b3, mask_res], ["attT"])
        b4 = nbank()
        for h in range(4):
            P.op(PE, (lambda h: lambda e: e.matmul(bk(b4)[:, h * 128:(h + 1) * 128], lhsT=v_b[:, h * 128:(h + 1) * 128],
                                                    rhs=attT[:, h, :], start=(h == 0), stop=False,
                                                    skip_group_check=True))(h),
                 ["v_b", "attT"], ["bank%d" % b4])
        for b in range(nseq):
            st = seq_states[b]
            if st.get("pre_o"):
                st["pre_o"]()
            Sb, rb = st["Sb"], st["rb"]
            for h in range(4):
                P.op(PE, (lambda h, b, Sb: lambda e: e.matmul(
                    bk(b4)[:, h * 128 + b * L:h * 128 + (b + 1) * L], lhsT=Sb(h), rhs=qtl[:, h, b * L:(b + 1) * L],
                    start=False, stop=(b == nseq - 1), skip_group_check=True))(h, b, Sb), [rb, "qtl"], ["bank%d" % b4])
        P.op(ACT, lambda e: e.activation(out=o_sb[:], in_=bk(b4)[:, :], func=AF.Copy), ["bank%d" % b4], ["o_sb"])
        P.op(ACT, lambda e: e.activation(out=sq_b[:], in_=bk(b4)[:, :], func=AF.Square), ["bank%d" % b4], ["sq_b"])
        b5 = nbank()
        P.op(PE, lambda e: e.matmul(bk(b5)[:, :], lhsT=ones_b[:], rhs=sq_b[:], start=True, stop=True),
             ["ones_b", "sq_b"], ["bank%d" % b5])
        P.op(DVE, lambda e: e.tensor_scalar(out=sd_f[:], in0=bk(b5)[:, :], scalar1=1.0 / 128.0, scalar2=EPS,
                                            op0=ALU.mult, op1=ALU.add), ["bank%d" % b5], ["sd_f"])
        P.op(ACT, lambda e: e.activation(out=sd_f[:], in_=sd_f[:], func=AF.Ln), ["sd_f"], ["sd_f"])
        P.op(ACT, lambda e: e.activation(out=sd_f[:], in_=sd_f[:], func=AF.Exp, scale=-0.5), ["sd_f"], ["sd_f"])
        P.op(DVE, lambda e: e.scalar_tensor_tensor(out=t1_f[:], in0=o_sb[:], scalar=ggh_c, in1=rs_f[:],
                                                   op0=ALU.mult, op1=ALU.mult), ["o_sb", "bgate_c", "sd_f"], ["o_sb"])
        P.op(DVE, lambda e: e.tensor_tensor(out=fl(ogT), in0=t1_f[:], in1=fl(rT), op=ALU.mult),
             ["o_sb", "rT"], ["ogT"])
        for b in range(nseq):
            st = seq_states[b]
            Sf, rf, use_mask = st["Sf"], st["rf"], st["use_mask"]
            if st.get("pre_s"):
                st["pre_s"]()
            if use_mask:
                vm = vm_b[b % 2]
                vres = "vm_b%d" % (b % 2)
                P.op(DVE, (lambda b, vm: lambda e: e.tensor_scalar(out=vm[:], in0=v_b[:], scalar1=bmask[:, b:b + 1],
                                                                   scalar2=None, op0=ALU.mult))(b, vm),
                     ["v_b", "cst_f"], [vres])
            else:
                vm = v_b
                vres = "v_b"
            b6 = nbank()
            for h in range(4):
                P.op(PE, (lambda h, vm, b6: lambda e: e.matmul(bk(b6)[:, h * 128:(h + 1) * 128],
                                                               lhsT=kh[:, h * 128:(h + 1) * 128],
                                                               rhs=vm[:, h * 128:(h + 1) * 128], start=True, stop=True))(h, vm, b6),
                     ["kh", vres], ["bank%d" % b6])
            for h in range(4):
                P.op(DVE, (lambda h, b, Sf, b6: lambda e: e.scalar_tensor_tensor(
                    out=Sf(h), in0=Sf(h), scalar=Epos[:, h, b * L + L - 1:b * L + L], in1=bk(b6)[:, h * 128:(h + 1) * 128],
                    op0=ALU.mult, op1=ALU.add))(h, b, Sf, b6), [rf, "Epos", "bank%d" % b6], [rf])
            if st.get("post_s"):
                st["post_s"]()

    negd = sb([128, NT, 8], F32, "negd"); Rsum = sb([128, 8], F32, "Rsum"); dend = sb([128, 8], F32, "dend")
    biasb = [sb([128, 8], F32, "biasb%d" % i) for i in range(2)]
    PT = [sb([128, 8, 128], BF16, "PT%d" % i) for i in range(2)]
    rinv = sb([128, 8], F32, "rinv")
    o_n = sb([128, 512], BF16, "o_n"); ofT = sb([128, 4, 128], BF16, "ofT"); omT = sb([128, 4, 128], BF16, "omT")
    P.op(DVE, lambda e: e.memset(Rsum[:], 0.0), [], ["Rsum"])
    pt_i = [0]
    qbd = sb([128, 8, 128], BF16, "qbd")
    P.op(DVE, lambda e: e.memset(qbd[:].rearrange("p h t -> p (h t)"), 0.0), [], ["qbd"])

    def build_qbd():
        qv = qbd[:].rearrange("p (c r) t -> p c r t", r=2)
        P.op(DVE, lambda e: e.tensor_copy(out=qv[0:64, :, 0, :], in_=qfT[0:64, :, :]), ["qfT"], ["qbd"])
        P.op(DVE, lambda e: e.tensor_copy(out=qv[64:128, :, 1, :], in_=qfT[64:128, :, :]), ["qfT"], ["qbd"])

    def fox_block(kt_ap_fn, kt_res, v_ap_fn, v_res, bias_ap, bias_res, mask_b, mask_res, first, last):
        k = pt_i[0] % 2
        pt_i[0] += 1
        ptb = PT[k]
        pres = "PT%d" % k
        for h in range(8):
            c = h // 2
            bkk = 4 + h // 4
            P.op(PE, (lambda h, c, bkk: lambda e: e.matmul(bk(bkk)[:, (h % 4) * 128:(h % 4 + 1) * 128],
                                                           lhsT=kt_ap_fn(c), rhs=qbd[:, h, :],
                                                           start=True, stop=True))(h, c, bkk),
                 [kt_res, "qbd"], ["bank%d" % bkk])
        for h in range(8):
            bkk = 4 + h // 4
            P.op(ACT, (lambda h, bkk: lambda e: e.activation(out=ptb[:, h, :], in_=bk(bkk)[:, (h % 4) * 128:(h % 4 + 1) * 128],
                                                             func=AF.Exp, scale=0.125, bias=bias_ap[:, h:h + 1]))(h, bkk),
                 ["bank%d" % bkk, bias_res], [pres])
        if mask_b is not None:
            P.op(DVE, lambda e: e.tensor_tensor(out=ptb[:], in0=ptb[:], in1=AP(mask_b, 0, [[0, 8], [1, 128]]),
                                                op=ALU.mult), [pres, mask_res], [pres])
        for h in range(8):
            bkk = 6 + h // 4
            P.op(PE, (lambda h, bkk: lambda e: e.matmul(bk(bkk)[:, (h % 4) * 66:(h % 4) * 66 + 66], lhsT=ptb[:, h, :],
                                                        rhs=v_ap_fn(h), start=(first and h % 4 == 0),
                                                        stop=last, skip_group_check=True))(h, bkk),
                 [pres, v_res], ["bank%d" % bkk])

    def attn_finish(nh, dh, dstT, dst_res):
        hp = nh // 2
        w = dh + 2
        for half in range(2):
            bkk = 6 + half
            v3 = bk(bkk)[:, 0:hp * w].rearrange("p (h d) -> p h d", d=w)
            P.op(DVE, (lambda v3, half: lambda e: e.reciprocal(out=rinv[:, half * hp:(half + 1) * hp],
                                                               in_=v3[:, :, dh]))(v3, half),
                 ["bank%d" % bkk], ["rinv"])
            P.op(DVE, (lambda v3, half: lambda e: e.tensor_tensor(
                out=o_n[:, half * 256:(half + 1) * 256].rearrange("p (h d) -> p h d", d=dh), in0=v3[:, :, 0:dh],
                in1=AP(rinv, half * hp, [[1, hp], [0, dh]]), op=ALU.mult))(v3, half),
                ["bank%d" % bkk, "rinv"], ["o_n"])
        transpose_rows(o_n, "o_n", dstT, dst_res, nchunk=4)

    def fox_prompt(i):
        build_qbd()
        b1 = nbank()
        P.op(PE, lambda e: e.matmul(bk(b1)[:, 0:8], lhsT=tri_f, rhs=lf_t[:], start=True, stop=False),
             ["cst_f", "lf_t"], ["bank%d" % b1])
        P.op(PE, lambda e: e.matmul(bk(b1)[:, 0:8], lhsT=ones_f[:], rhs=Rsum[:], start=False, stop=True),
             ["ones_f", "Rsum"], ["bank%d" % b1])
        P.op(DVE, lambda e: e.tensor_scalar(out=negd[:, i, :], in0=bk(b1)[:, 0:8], scalar1=-1.0, scalar2=None,
                                            op0=ALU.mult), ["bank%d" % b1], ["negd%d" % i])
        P.op(DVE, lambda e: e.tensor_tensor(out=Rsum[:], in0=Rsum[:], in1=lf_t[:], op=ALU.add),
             ["Rsum", "lf_t"], ["Rsum"])
        b2 = nbank()
        P.op(PE, lambda e: e.matmul(bk(b2)[:, 0:8], lhsT=ones_f[:], rhs=Rsum[:], start=True, stop=True),
             ["ones_f", "Rsum"], ["bank%d" % b2])
        P.op(DVE, lambda e: e.tensor_copy(out=dend[:], in_=bk(b2)[:, 0:8]), ["bank%d" % b2], ["dend"])
        for j in range(i + 1):
            bb = biasb[j % 2]
            bres = "biasb%d" % (j % 2)
            P.op(DVE, (lambda j, bb: lambda e: e.tensor_tensor(out=bb[:], in0=dend[:], in1=negd[:, j, :], op=ALU.add))(j, bb),
                 ["dend", "negd%d" % j], [bres])
            fox_block(lambda c, j=j: KT[:, c, j * 128:(j + 1) * 128], "KT%d" % j,
                      lambda h, j=j: Vaug[:, j, h, :], "Vaug%d" % j, bb, bres,
                      tri_b if j == i else None, "tri_b", j == 0, j == i)
        attn_finish(8, 64, ofT, "ofT")

    mkT = sb([128, 4, 256], BF16, "mkT"); mvA = sb([128, 2, 4, 130], BF16, "mvA")
    PTm = PT
    mk_tm = fk_tm; mv_tm = fv_tm
    mnT = nT
    P.op(DVE, lambda e: e.memset(mvA[:].rearrange("p a h d -> p (a h d)"), 1.0), [], ["mvA"])
    w_mkv_v = w_mem_kv.rearrange("(c p) n -> p c n", p=128)

    def mem_setup_prompt():
        for mb in range(2):
            P.dma(SP, (lambda mb: lambda e: e.dma_start(out=x_t[:], in_=memp[mb * 128:(mb + 1) * 128, :]))(mb), [], ["x_t"])
            rmsnorm_rows(x_t[:], "x_t", gmem_bc, "acc", n_b[:], "n_b")
            transpose_rows(n_b, "n_b", mnT, "nT")

            def ev_k(ps, res, mb=mb):
                P.op(ACT, lambda e: e.activation(out=mk_tm[:], in_=ps, func=AF.Copy), [res], ["fk_tm"])
                P.dma(SP, lambda e: e.dma_start(out=mkp[mb * 128:(mb + 1) * 128, :], in_=mk_tm[:]), ["fk_tm"], [ores()])
            proj_tm(w_mkv_v[:, :, 0:512], 512, ev_k, mnT, "nT")

            def ev_v(ps, res, mb=mb):
                P.op(ACT, lambda e: e.activation(out=mv_tm[:], in_=ps, func=AF.Copy), [res], ["fv_tm"])
                P.op(DVE, lambda e: e.tensor_copy(out=mvA[:, mb, :, 0:128], in_=mv_tm[:].rearrange("p (h d) -> p h d", h=4)),
                     ["fv_tm"], ["mvA"])
                P.dma(SP, lambda e: e.dma_start(out=mvp[mb * 128:(mb + 1) * 128, :], in_=mv_tm[:]), ["fv_tm"], [ores()])
            proj_tm(w_mkv_v[:, :, 512:1024], 512, ev_v, mnT, "nT")

            def ev_kT(ps, res, mb=mb):
                P.op(ACT, lambda e: e.activation(out=mkT[:, :, mb * 128:(mb + 1) * 128],
                                                 in_=ps.rearrange("p (c t) -> p c t", c=4), func=AF.Copy), [res], ["mkT"])
            proj_fm(w_mkv_v[:, :, 0:512], 512, ev_kT, mnT, "nT")

    def mem_attend_block(mkT_fn, mk_res, mv_fn, mv_res, ptm, ptm_res, out_ap_fn, cols, first, last):
        c0, n = cols
        for h in range(4):
            for mb in range(2):
                idx = h * 2 + mb
                bkk = 4 + idx // 4
                P.op(PE, (lambda h, mb, idx, bkk: lambda e: e.matmul(bk(bkk)[:, (idx % 4) * 128:(idx % 4) * 128 + n],
                                                                      lhsT=mkT_fn(h, mb), rhs=qmT[:, h, c0:c0 + n],
                                                                      start=True, stop=True))(h, mb, idx, bkk),
                     [mk_res, "qmT"], ["bank%d" % bkk])
        for half in range(2):
            bkk = 4 + half
            P.op(ACT, (lambda half, bkk: lambda e: e.activation(
                out=ptm[:, half * 4:(half + 1) * 4, c0:c0 + n],
                in_=bk(bkk)[:, :].rearrange("p (i t) -> p i t", i=4)[:, :, 0:n], func=AF.Exp, scale=128.0 ** -0.5))(half, bkk),
                ["bank%d" % bkk], [ptm_res])
        for h in range(4):
            for mb in range(2):
                bkk = 6 + h // 2
                P.op(PE, (lambda h, mb, bkk: lambda e: e.matmul(bk(bkk)[:, (h % 2) * 130:(h % 2) * 130 + 130],
                                                                lhsT=ptm[:, h * 2 + mb, :], rhs=mv_fn(h, mb),
                                                                start=(first and h % 2 == 0 and mb == 0),
                                                                stop=(last and mb == 1), skip_group_check=True))(h, mb, bkk),
                     [ptm_res, mv_res], ["bank%d" % bkk])

    m_acc = sb([128, 512], F32, "m_acc"); m_tmp = sb([128, 512], F32, "m_tmp")
    mT = sb([128, 8, 128], BF16, "mT"); h_t = x_t
    wo_views = [w.rearrange("(c p) n -> p c n", p=128) for w in (w_gla_o, w_fox_o, w_mem_o)]
    w_out_v = w_out.rearrange("(c p) n -> p c n", p=128)
    brT = [(ogT, "ogT"), (ofT, "ofT"), (omT, "omT")]

    def merge_out():
        for half in range(2):
            pbs = []
            for b in range(3):
                wv, wn = wload(wo_views[b][:, :, half * 512:(half + 1) * 512], (4, 512))
                bkk = 1 + b
                for cc in range(4):
                    for kc in range(4):
                        P.op(PE, (lambda b, cc, kc, wv, bkk: lambda e: e.matmul(
                            bk(bkk)[:, cc * 128:(cc + 1) * 128], lhsT=wv[:, kc, cc * 128:(cc + 1) * 128],
                            rhs=brT[b][0][:, kc, :], start=(kc == 0), stop=(kc == 3)))(b, cc, kc, wv, bkk),
                            [wn, brT[b][1]], ["bank%d" % bkk])
            gl = lambda b: gatesT[:, b * 8 + half * 4:b * 8 + half * 4 + 4, :].rearrange("p c t -> p (c t)")
            gres = lambda b: ["gatesT%d" % (b * 8 + half * 4 + j) for j in range(4)]
            g0, g1, g2 = gl(0), gl(1), gl(2)
            P.op(DVE, (lambda g0: lambda e: e.tensor_tensor(out=m_acc[:], in0=bk(1)[:, :], in1=g0, op=ALU.mult))(g0),
                 ["bank1"] + gres(0), ["m_acc"])
            P.op(DVE, (lambda g1: lambda e: e.tensor_tensor(out=m_tmp[:], in0=bk(2)[:, :], in1=g1, op=ALU.mult))(g1),
                 ["bank2"] + gres(1), ["m_tmp"])
            P.op(DVE, lambda e: e.tensor_tensor(out=m_acc[:], in0=m_acc[:], in1=m_tmp[:], op=ALU.add),
                 ["m_acc", "m_tmp"], ["m_acc"])
            P.op(DVE, (lambda g2: lambda e: e.tensor_tensor(out=m_tmp[:], in0=bk(3)[:, :], in1=g2, op=ALU.mult))(g2),
                 ["bank3"] + gres(2), ["m_tmp"])
            P.op(DVE, (lambda half: lambda e: e.tensor_tensor(
                out=mT[:, half * 4:(half + 1) * 4, :].rearrange("p c t -> p (c t)"), in0=m_acc[:], in1=m_tmp[:],
                op=ALU.add))(half), ["m_acc", "m_tmp"], ["mT"])
        for half in range(2):
            wv, wn = wload(w_out_v[:, :, half * 512:(half + 1) * 512], (8, 512))
            bkk = 4 + half
            for kc in range(8):
                P.op(PE, (lambda kc, wv, bkk: lambda e: e.matmul(bk(bkk)[:, :], lhsT=mT[:, kc, :], rhs=wv[:, kc, :],
                                                                 start=(kc == 0), stop=(kc == 7)))(kc, wv, bkk),
                     ["mT", wn], ["bank%d" % bkk])
            P.op(DVE, (lambda half, bkk: lambda e: e.tensor_tensor(out=h_t[:, half * 512:(half + 1) * 512],
                                                                   in0=x_t[:, half * 512:(half + 1) * 512],
                                                                   in1=bk(bkk)[:, :], op=ALU.add))(half, bkk),
                 ["x_t", "bank%d" % bkk], ["x_t"])

    xn_b = n_b; xnT = nT
    qpT = sb([128, 16, 128], BF16, "qpT")
    k1T = sb([128, 128], BF16, "k1T"); k2T = sb([128, 128], BF16, "k2T")
    v16 = sb([128, 16, 16], F32, "v16"); i16 = sb([128, 16, 16], U32, "i16"); i16f = sb([128, 16, 16], F32, "i16f")
    work = sb([128, 128], F32, "work"); cand = sb([128, 8, 256], F32, "cand"); work2 = sb([128, 256], F32, "work2")
    sc16 = sb([128, 8, 16], F32, "sc16"); pos = sb([128, 8, 16], U32, "pos")
    aj_u = sb([128, 8, 16], U32, "aj_u"); bj_u = sb([128, 8, 16], U32, "bj_u")
    aj_f = sb([128, 8, 16], F32, "aj_f"); bj_f = sb([128, 8, 16], F32, "bj_f")
    oh = sb([128, 8, 16, 16], BF16, "oh")
    i1s = sb([128, 8, 16], F32, "i1s"); i2s = sb([128, 8, 16], F32, "i2s")
    eidx_f = sb([128, 128], F32, "eidx_f"); eidx_u = sb([128, 128], U32, "eidx_u")
    e16 = sb([128, 8, 16], F32, "e16"); zs = sb([128, 8], F32, "zs"); g16 = sb([128, 128], F32, "g16")
    a_t = sb([128, 128], F32, "a_t"); a2 = sb([128, 128], F32, "a2"); wgt = sb([128, 128], F32, "wgt")
    NSLOT = 4
    Ug = [sb([128, D], BF16, "Ug%d" % i) for i in range(NSLOT)]
    Vg = Ug
    w_pq_v = w_pq.rearrange("(c p) n -> p c n", p=128)

    def peer_setup():
        for src, dst, nm in ((pk1, k1T, "k1T"), (pk2, k2T, "k2T")):
            P.dma(SP, (lambda src: lambda e: e.dma_start(out=work[:], in_=src[:, :]))(src), [], ["work"])
            P.op(PE, lambda e: e.transpose(out=bk(0)[:, 0:128], in_=work[:], identity=ident_f), ["work", "cst_f"], ["bank0"])
            P.op(ACT, (lambda dst: lambda e: e.activation(out=dst[:], in_=bk(0)[:, 0:128], func=AF.Copy))(dst),
                 ["bank0"], [nm])

    def peer(y_out):
        rmsnorm_rows(h_t[:], "x_t", gffn_bc, "gffn_bc", xn_b[:], "n_b")
        transpose_rows(xn_b, "n_b", xnT, "nT")
        for blk in range(4):
            def ev_q(ps, res, blk=blk):
                P.op(ACT, lambda e: e.activation(out=qpT[:, blk * 4:(blk + 1) * 4, :].rearrange("p c t -> p (c t)"),
                                                 in_=ps, func=AF.Copy), [res], ["qpT"])
            proj_fm(w_pq_v[:, :, blk * 512:(blk + 1) * 512], 512, ev_q, xnT, "nT")
        for c in range(16):
            bkk = 4 + c // 4
            kT_ = k1T if c % 2 == 0 else k2T
            P.op(PE, (lambda c, bkk, kT_: lambda e: e.matmul(bk(bkk)[:, (c % 4) * 128:(c % 4 + 1) * 128], lhsT=qpT[:, c, :],
                                                             rhs=kT_[:], start=True, stop=True))(c, bkk, kT_),
                 ["qpT", "k1T", "k2T"], ["bank%d" % bkk])
        scv = lambda c: bk(4 + c // 4)[:, (c % 4) * 128:(c % 4 + 1) * 128]
        scr = lambda c: "bank%d" % (4 + c // 4)
        for c in range(16):
            P.op(DVE, (lambda c: lambda e: e.max(out=v16[:, c, 0:8], in_=scv(c)))(c), [scr(c)], ["v16"])
            P.op(DVE, (lambda c: lambda e: e.max_index(out=i16[:, c, 0:8], in_max=v16[:, c, 0:8], in_values=scv(c)))(c),
                 [scr(c), "v16"], ["i16"])
            P.op(DVE, (lambda c: lambda e: e.match_replace(out=work[:], in_to_replace=v16[:, c, 0:8], in_values=scv(c),
                                                           imm_value=NEG))(c), [scr(c), "v16"], ["work"])
            P.op(DVE, (lambda c: lambda e: e.max(out=v16[:, c, 8:16], in_=work[:]))(c), ["work"], ["v16"])
            P.op(DVE, (lambda c: lambda e: e.max_index(out=i16[:, c, 8:16], in_max=v16[:, c, 8:16], in_values=work[:]))(c),
                 ["work", "v16"], ["i16"])
        fl3 = lambda t: t[:].rearrange("p a b -> p (a b)")
        P.op(DVE, lambda e: e.tensor_copy(out=fl3(i16f), in_=fl3(i16)), ["i16"], ["i16f"])
        P.op(DVE, lambda e: e.tensor_tensor(out=cand[:].rearrange("p h (a b) -> p h a b", a=16),
                                            in0=AP(v16, 0, [[32, 8], [1, 16], [0, 16]]),
                                            in1=AP(v16, 16, [[32, 8], [0, 16], [1, 16]]), op=ALU.add), ["v16"], ["cand"])
        for h in range(8):
            P.op(DVE, (lambda h: lambda e: e.max(out=sc16[:, h, 0:8], in_=cand[:, h, :]))(h), ["cand"], ["sc16"])
            P.op(DVE, (lambda h: lambda e: e.max_index(out=pos[:, h, 0:8], in_max=sc16[:, h, 0:8], in_values=cand[:, h, :]))(h),
                 ["cand", "sc16"], ["pos"])
            P.op(DVE, (lambda h: lambda e: e.match_replace(out=work2[:], in_to_replace=sc16[:, h, 0:8],
                                                           in_values=cand[:, h, :], imm_value=NEG))(h),
                 ["cand", "sc16"], ["work2"])
            P.op(DVE, (lambda h: lambda e: e.max(out=sc16[:, h, 8:16], in_=work2[:]))(h), ["work2"], ["sc16"])
            P.op(DVE, (lambda h: lambda e: e.max_index(out=pos[:, h, 8:16], in_max=sc16[:, h, 8:16], in_values=work2[:]))(h),
                 ["work2", "sc16"], ["pos"])
        P.op(DVE, lambda e: e.tensor_single_scalar(out=fl3(aj_u), in_=fl3(pos), scalar=4, op=ALU.logical_shift_right),
             ["pos"], ["aj_u"])
        P.op(DVE, lambda e: e.tensor_single_scalar(out=fl3(bj_u), in_=fl3(pos), scalar=15, op=ALU.bitwise_and),
             ["pos"], ["bj_u"])
        P.op(DVE, lambda e: e.tensor_copy(out=fl3(aj_f), in_=fl3(aj_u)), ["aj_u"], ["aj_f"])
        P.op(DVE, lambda e: e.tensor_copy(out=fl3(bj_f), in_=fl3(bj_u)), ["bj_u"], ["bj_f"])
        for sel_f, sres, off, dst, dres in ((aj_f, "aj_f", 0, i1s, "i1s"), (bj_f, "bj_f", 16, i2s, "i2s")):
            P.op(DVE, (lambda sel_f: lambda e: e.tensor_tensor(out=oh[:], in0=AP(cst_f, 512, [[0, 8], [0, 16], [1, 16]]),
                                                               in1=AP(sel_f, 0, [[16, 8], [1, 16], [0, 16]]),
                                                               op=ALU.is_equal))(sel_f), ["cst_f", sres], ["oh"])
            P.op(DVE, (lambda off: lambda e: e.tensor_tensor(out=oh[:], in0=oh[:], in1=AP(i16f, off, [[32, 8], [0, 16], [1, 16]]),
                                                             op=ALU.mult))(off), ["oh", "i16f"], ["oh"])
            P.op(DVE, (lambda dst: lambda e: e.tensor_reduce(out=dst[:], in_=oh[:], axis=AX.X, op=ALU.add))(dst),
                 ["oh"], [dres])
        P.op(DVE, lambda e: e.scalar_tensor_tensor(out=eidx_f[:], in0=fl3(i1s), scalar=128.0, in1=fl3(i2s),
                                                   op0=ALU.mult, op1=ALU.add), ["i1s", "i2s"], ["eidx_f"])
        P.op(DVE, lambda e: e.tensor_scalar(out=eidx_f[:], in0=eidx_f[:], scalar1=0.0, scalar2=16383.0,
                                            op0=ALU.max, op1=ALU.min), ["eidx_f"], ["eidx_f"])
        P.op(DVE, lambda e: e.tensor_copy(out=eidx_u[:], in_=eidx_f[:]), ["eidx_f"], ["eidx_u"])
        P.op(DVE, lambda e: e.tensor_tensor(out=e16[:], in0=sc16[:], in1=AP(sc16, 0, [[16, 8], [0, 16]]), op=ALU.subtract),
             ["sc16"], ["e16"])
        P.op(ACT, lambda e: e.activation(out=fl3(e16), in_=fl3(e16), func=AF.Exp), ["e16"], ["e16"])
        P.op(DVE, lambda e: e.tensor_reduce(out=zs[:], in_=e16[:], axis=AX.X, op=ALU.add), ["e16"], ["zs"])
        P.op(DVE, lambda e: e.reciprocal(out=zs[:], in_=zs[:]), ["zs"], ["zs"])
        P.op(DVE, lambda e: e.tensor_tensor(out=g16[:].rearrange("p (h k) -> p h k", h=8), in0=e16[:],
                                            in1=AP(zs, 0, [[1, 8], [0, 16]]), op=ALU.mult), ["e16", "zs"], ["g16"])
        for hj in range(128):
            s = hj % NSLOT
            P.dma(POOL, (lambda hj, s: lambda e: e.indirect_dma_start(
                out=Ug[s][:], out_offset=None, in_=pu[:, :],
                in_offset=bass.IndirectOffsetOnAxis(ap=eidx_u[:, hj:hj + 1], axis=0)))(hj, s), ["eidx_u"], ["Ug%d" % s])
            P.op(DVE, (lambda hj, s: lambda e: e.scalar_tensor_tensor(
                out=junk_b[:], in0=Ug[s][:], scalar=1.0, in1=xn_b[:], op0=ALU.mult, op1=ALU.mult,
                accum_out=a_t[:, hj:hj + 1]))(hj, s), ["Ug%d" % s, "n_b"], ["junk_b", "a_t"])
        P.op(DVE, lambda e: e.tensor_tensor(out=a2[:], in0=a_t[:], in1=a_t[:], op=ALU.mult), ["a_t"], ["a2"])
        P.op(DVE, lambda e: e.tensor_scalar(out=a2[:], in0=a2[:], scalar1=0.044715, scalar2=1.0, op0=ALU.mult, op1=ALU.add),
             ["a2"], ["a2"])
        P.op(DVE, lambda e: e.tensor_tensor(out=a2[:], in0=a2[:], in1=a_t[:], op=ALU.mult), ["a2", "a_t"], ["a2"])
        P.op(ACT, lambda e: e.activation(out=a2[:], in_=a2[:], func=AF.Sigmoid, scale=1.5957691216057308), ["a2"], ["a2"])
        P.op(DVE, lambda e: e.tensor_tensor(out=a2[:], in0=a2[:], in1=a_t[:], op=ALU.mult), ["a2", "a_t"], ["a2"])
        P.op(DVE, lambda e: e.tensor_tensor(out=wgt[:], in0=a2[:], in1=g16[:], op=ALU.mult), ["a2", "g16"], ["wgt"])
        P.op(DVE, lambda e: e.memset(acc[:], 0.0), [], ["acc"])
        for hj in range(128):
            s = hj % NSLOT
            P.dma(POOL, (lambda hj, s: lambda e: e.indirect_dma_start(
                out=Vg[s][:], out_offset=None, in_=pv[:, :],
                in_offset=bass.IndirectOffsetOnAxis(ap=eidx_u[:, hj:hj + 1], axis=0)))(hj, s), ["eidx_u"], ["Ug%d" % s])
            P.op(DVE, (lambda hj, s: lambda e: e.scalar_tensor_tensor(out=acc[:], in0=Vg[s][:], scalar=wgt[:, hj:hj + 1],
                                                                      in1=acc[:], op0=ALU.mult, op1=ALU.add))(hj, s),
                 ["Ug%d" % s, "wgt", "acc"], ["acc"])
        P.op(DVE, lambda e: e.tensor_tensor(out=acc[:], in0=acc[:], in1=h_t[:], op=ALU.add), ["acc", "x_t"], ["acc"])
        rmsnorm_rows(acc[:], "acc", gfin_bc, "gfin_bc", acc[:], "acc")
        P.dma(SP, lambda e: e.dma_start(out=y_out, in_=acc[:]), ["acc"], [ores()])

    IOA = bass.IndirectOffsetOnAxis

    def sample_tile():
        Qbd = sb([128, 16, 4, 16], BF16, "Qbd")
        idx_u = sb([128, 256], U32, "idx_u"); pt_b = sb([128, 256], I32, "pt_b"); ptf = sb([128, 256], F32, "ptf")
        pt_pair = sb([128, 2], I32, "pt_pair")
        tot = sb([128, 2, 8], F32, "tot"); base = sb([128, 2, 8], F32, "base")
        nb_bias = sb([128, 8], F32, "nb_bias")
        PTs = sb([128, 1024], BF16, "PTs")
        Kpg = [sb([128, 512], BF16, "Kpg%d" % i) for i in range(2)]
        Vraw = [sb([128, 512], BF16, "Vraw%d" % i) for i in range(2)]
        o_un = sb([8, 528], F32, "o_un")
        lfP = VaugR[:].bitcast(F32)[:, 0:2048].rearrange("p (a n) -> p a n", a=2)
        Pb = oh[:].rearrange("p a b c -> p (a b c)").bitcast(F32).rearrange("p (h j) -> p h j", h=8)
        bias_s = cand[:].rearrange("p a b -> p (a b)").rearrange("p (n h) -> p n h", h=8)
        sc_s = acc
        Sfb = [m_acc, m_tmp]
        Sfr = ["m_acc", "m_tmp"]

        P.dma(SP, lambda e: e.dma_start(out=pt_b[:], in_=bc_rows(pt, 256)), [], ["pt_b"])
        P.dma(SP, lambda e: e.dma_start(out=pt_pair[:], in_=pt.rearrange("o (a p) -> p (o a)", p=128),
                                        allow_slow_non_contiguous=True), [], ["pt_pair"])
        P.op(DVE, lambda e: e.tensor_copy(out=ptf[:], in_=pt_b[:]), ["pt_b"], ["ptf"])
        P.op(DVE, lambda e: e.tensor_scalar(out=ptf[:], in0=ptf[:], scalar1=128.0, scalar2=cst_f[:, 544:545],
                                            op0=ALU.mult, op1=ALU.add), ["ptf", "cst_f"], ["ptf"])
        P.op(DVE, lambda e: e.tensor_copy(out=idx_u[:], in_=ptf[:]), ["ptf"], ["idx_u"])

        front(xs[:, :], 0, True)

        sg_k = sg.rearrange("n k v -> k n v")
        gss_k = gss.rearrange("n k v -> k n v")
        states = []
        for b in range(16):
            slot = (b // 2) % NSLOT
            bl = b % 2

            def pre_o(b=b, slot=slot):
                if b % 2 == 0:
                    dst = Ug[slot][:, :].rearrange("p (n v) -> p n v", n=8)
                    P.dma(POOL, lambda e: e.dma_start(out=dst, in_=sg_k[:, b * 4:b * 4 + 8, :]), [], ["Ug%d" % slot])

            def pre_s(b=b):
                dst = Sfb[b % 2][:, :].rearrange("p (n v) -> p n v", n=4)
                P.dma(SP, lambda e: e.dma_start(out=dst, in_=sg_k[:, b * 4:b * 4 + 4, :]), [], [Sfr[b % 2]])

            def post_s(b=b):
                src = Sfb[b % 2][:, :].rearrange("p (n v) -> p n v", n=4)
                P.dma(SP, lambda e: e.dma_start(out=gss_k[:, b * 4:b * 4 + 4, :], in_=src), [Sfr[b % 2]], [ores()])

            states.append(dict(
                Sf=(lambda h, b=b: Sfb[b % 2][:, h * 128:(h + 1) * 128]), rf=Sfr[b % 2],
                Sb=(lambda h, slot=slot, bl=bl: Ug[slot][:, (bl * 4 + h) * 128:(bl * 4 + h + 1) * 128]),
                rb="Ug%d" % slot, use_mask=True, pre_o=pre_o, pre_s=pre_s, post_s=post_s))
        if stage >= 2:
            gla(8, nbtri16, "nbtri16", btri_b, "btri_b", states)

        if stage >= 2:
            clf_pg = clf.rearrange("(n j) h -> n (j h)", j=128)
            for half in range(2):
                P.dma(POOL, (lambda half: lambda e: e.indirect_dma_start(
                    out=lfP[:, half, :], out_offset=None, in_=clf_pg[:, :],
                    in_offset=IOA(ap=pt_pair[:, half:half + 1].bitcast(U32), axis=0)))(half), ["pt_pair"], VAUG_ALL)
            for half in range(2):
                lv = lfP[:, half, :].rearrange("p (j h) -> p h j", h=8)
                P.op(DVE, (lambda half, lv: lambda e: e.tensor_reduce(out=tot[:, half, :], in_=lv, axis=AX.X, op=ALU.add))(half, lv),
                     VAUG_ALL, ["tot"])
                b1 = nbank()
                P.op(PE, (lambda half, b1: lambda e: e.matmul(bk(b1)[:, 0:8], lhsT=Mst_f, rhs=tot[:, half, :],
                                                              start=True, stop=False))(half, b1),
                     ["cst2_f", "tot"], ["bank%d" % b1])
                P.op(PE, (lambda half, b1: lambda e: e.matmul(bk(b1)[:, 0:8], lhsT=Bsel_f[half], rhs=lf_t[:],
                                                              start=False, stop=True))(half, b1),
                     ["cst2_f", "lf_t"], ["bank%d" % b1])
                P.op(DVE, (lambda half, b1: lambda e: e.tensor_tensor(out=base[:, half, :], in0=tot[:, half, :],
                                                                      in1=bk(b1)[:, 0:8], op=ALU.add))(half, b1),
                     ["tot", "bank%d" % b1], ["base"])
                for h in range(8):
                    P.op(DVE, (lambda h, lv: lambda e: e.tensor_tensor_scan(out=Pb[:, h, :], data0=ones_f[:, :], data1=lv[:, h, :],
                                                                            initial=0.0, op0=ALU.mult, op1=ALU.add))(h, lv),
                         VAUG_ALL + ["ones_f"], ["oh"])
                P.op(DVE, (lambda half: lambda e: e.tensor_tensor(out=Pb, in0=AP(base, half * 8, [[1, 8], [0, 128]]), in1=Pb,
                                                                  op=ALU.subtract))(half), ["base", "oh"], ["oh"])
                for h in range(8):
                    bkk = 4 + h // 4
                    P.op(PE, (lambda h, bkk: lambda e: e.transpose(out=bk(bkk)[:, (h % 4) * 128:(h % 4 + 1) * 128],
                                                                   in_=Pb[:, h, :], identity=ident_f))(h, bkk),
                         ["oh", "cst_f"], ["bank%d" % bkk])
                for q in range(2):
                    P.op(ACT, (lambda q, half: lambda e: e.activation(
                        out=bias_s[:, half * 128:(half + 1) * 128, q * 4:(q + 1) * 4],
                        in_=bk(4 + q)[:, :].rearrange("p (h t) -> p t h", h=4), func=AF.Copy))(q, half),
                        ["bank%d" % (4 + q)], ["cand"])
            for q in range(2):
                P.op(DVE, (lambda q: lambda e: e.memset(VaugR[:, q * 2112:(q + 1) * 2112], 1.0))(q), [], VAUG_ALL)
            b1 = nbank()
            P.op(PE, lambda e: e.matmul(bk(b1)[:, 0:8], lhsT=bustr8_f, rhs=lf_t[:], start=True, stop=True),
                 ["cst2_f", "lf_t"], ["bank%d" % b1])
            P.op(DVE, lambda e: e.tensor_copy(out=nb_bias[:], in_=bk(b1)[:, 0:8]), ["bank%d" % b1], ["nb_bias"])
            P.op(DVE, lambda e: e.memset(Qbd[:].rearrange("p b c q -> p (b c q)"), 0.0), [], ["Qbd"])
            for c in range(4):
                P.op(DVE, (lambda c: lambda e: e.tensor_copy(out=Qbd[0:64, :, c, 0:8],
                                                             in_=qfT[0:64, c, :].rearrange("p (b q) -> p b q", q=8)))(c),
                     ["qfT"], ["Qbd"])
                P.op(DVE, (lambda c: lambda e: e.tensor_copy(out=Qbd[64:128, :, c, 8:16],
                                                             in_=qfT[64:128, c, :].rearrange("p (b q) -> p b q", q=8)))(c),
                     ["qfT"], ["Qbd"])
            build_qbd()
            fox_block(lambda c: KTn[:, c, :], "KTn", lambda h: Vn[:, h, :], "Vn", nb_bias, "nb_bias",
                      btri_b, "btri_b", True, False)
            kk = 0
            pb0 = bk_bf(0)
            for b in range(16):
                for g in range(16):
                    pair = b * 16 + g
                    k = kk % 2
                    kk += 1
                    P.dma(POOL, (lambda k, pair: lambda e: e.indirect_dma_start(
                        out=Kpg[k][:], out_offset=None, in_=ck[:, :],
                        in_offset=IOA(ap=idx_u[:, pair:pair + 1], axis=0)))(k, pair), ["idx_u"], ["Kpg%d" % k])
                    P.dma(POOL, (lambda k, pair: lambda e: e.indirect_dma_start(
                        out=Vraw[k][:], out_offset=None, in_=cv[:, :],
                        in_offset=IOA(ap=idx_u[:, pair:pair + 1], axis=0)))(k, pair), ["idx_u"], ["Vraw%d" % k])
                    for c in range(4):
                        P.op(PE, (lambda k, c: lambda e: e.transpose(out=pb0[:, k * 512 + c * 128:k * 512 + (c + 1) * 128],
                                                                     in_=Kpg[k][:, c * 128:(c + 1) * 128], identity=ident_b[:]))(k, c),
                             ["Kpg%d" % k, "ident_b"], ["bank0"])
                    P.op(ACT, (lambda k, g: lambda e: e.activation(
                        out=KT[:, :, g * 128:(g + 1) * 128],
                        in_=pb0[:, k * 512:(k + 1) * 512].rearrange("p (c t) -> p c t", c=4), func=AF.Copy))(k, g),
                        ["bank0"], ["KT%d" % g])
                    P.op(DVE, (lambda k, g: lambda e: e.tensor_copy(out=Vaug[:, g, :, 0:64],
                                                                    in_=Vraw[k][:, :].rearrange("p (h d) -> p h d", h=8)))(k, g),
                         ["Vraw%d" % k], ["Vaug%d" % g])
                    for c in range(4):
                        bkk = 4 + g // 8
                        P.op(PE, (lambda g, c, b, bkk: lambda e: e.matmul(
                            bk(bkk)[:, (g % 8) * 64 + c * 16:(g % 8) * 64 + (c + 1) * 16], lhsT=KT[:, c, g * 128:(g + 1) * 128],
                            rhs=Qbd[:, b, c, :], start=True, stop=True))(g, c, b, bkk),
                            ["KT%d" % g, "Qbd"], ["bank%d" % bkk])
                for gh in range(2):
                    P.op(DVE, (lambda gh, b: lambda e: e.scalar_tensor_tensor(
                        out=sc_s[:, gh * 512:(gh + 1) * 512].rearrange("p (n q) -> p n q", q=8),
                        in0=bk(4 + gh)[:, :].rearrange("p (n q) -> p n q", q=8), scalar=0.125,
                        in1=AP(cand, b * 128 + gh * 64, [[1, 64], [0, 8]]), op0=ALU.mult, op1=ALU.add))(gh, b),
                        ["bank%d" % (4 + gh), "cand"], ["acc"])
                P.op(ACT, lambda e: e.activation(out=PTs[:], in_=sc_s[:], func=AF.Exp), ["acc"], ["PTs"])
                for g in range(16):
                    for h in range(8):
                        bkk = 1 + h // 4
                        P.op(PE, (lambda g, h, bkk: lambda e: e.matmul(
                            bk(bkk)[0:8, (h % 4) * 66:(h % 4) * 66 + 66], lhsT=PTs[:, g * 64 + h * 8:g * 64 + h * 8 + 8],
                            rhs=Vaug[:, g, h, :], start=(g == 0 and h % 4 == 0), stop=(g == 15),
                            skip_group_check=True))(g, h, bkk), ["PTs", "Vaug%d" % g], ["bank%d" % bkk])
                for q in range(2):
                    P.op(ACT, (lambda q: lambda e: e.activation(out=o_un[0:8, q * 264:(q + 1) * 264], in_=bk(1 + q)[0:8, 0:264],
                                                                func=AF.Copy))(q), ["bank%d" % (1 + q)], ["o_un"])
                for q in range(2):
                    P.op(PE, (lambda q, b: lambda e: e.matmul(
                        bk(6 + q)[:, 0:264], lhsT=Esel_f[0:8, 120 - 8 * b:248 - 8 * b], rhs=o_un[0:8, q * 264:(q + 1) * 264],
                        start=False, stop=(b == 15), skip_group_check=True))(q, b), ["cst2_f", "o_un"], ["bank%d" % (6 + q)])
            attn_finish(8, 64, ofT, "ofT")

        if stage >= 3:
            for k in range(2):
                P.op(DVE, (lambda k: lambda e: e.memset(PTm[k][:].rearrange("p a t -> p (a t)"), 0.0))(k), [], ["PT%d" % k])
            pb0 = bk_bf(0)
            for b in range(16):
                k = b % 2
                mkb, mkn = wload(cmk[b].rearrange("(mb p) n -> p mb n", p=128), (2, 512))
                mvb, mvn = wload(cmv[b].rearrange("(mb p) n -> p mb n", p=128), (2, 512))
                for mb in range(2):
                    for h in range(4):
                        P.op(PE, (lambda mb, h, mkb: lambda e: e.transpose(
                            out=pb0[:, (mb * 4 + h) * 128:(mb * 4 + h + 1) * 128], in_=mkb[:, mb, h * 128:(h + 1) * 128],
                            identity=ident_b[:]))(mb, h, mkb), [mkn, "ident_b"], ["bank0"])
                    P.op(ACT, (lambda mb: lambda e: e.activation(
                        out=mkT[:, :, mb * 128:(mb + 1) * 128],
                        in_=pb0[:, mb * 512:(mb + 1) * 512].rearrange("p (c t) -> p c t", c=4), func=AF.Copy))(mb),
                        ["bank0"], ["mkT"])
                P.op(DVE, (lambda mvb: lambda e: e.tensor_copy(out=mvA[:, :, :, 0:128],
                                                               in_=mvb.rearrange("p mb (h d) -> p mb h d", h=4)))(mvb),
                     [mvn], ["mvA"])
                mem_attend_block(lambda h, mb: mkT[:, h, mb * 128:(mb + 1) * 128], "mkT",
                                 lambda h, mb: mvA[:, mb, h, :], "mvA", PTm[k], "PT%d" % k, None, (b * 8, 8), b == 0, b == 15)
                P.op(DVE, (lambda k, b: lambda e: e.memset(PTm[k][:, :, b * 8:(b + 1) * 8], 0.0))(k, b), [], ["PT%d" % k])
            attn_finish(4, 128, omT, "omT")
        if stage >= 4:
            merge_out()
        if stage >= 5:
            peer(ys[:, :])

    prompt_state = [dict(Sf=lambda h: S_f[:, h, :], rf="S_f", Sb=lambda h: S_b[:, h, :], rb="S_b", use_mask=False)]
    if stage >= 3:
        mem_setup_prompt()
    if stage >= 5:
        peer_setup()
    for ti in range(ntiles):
        front(xp[ti * 128:(ti + 1) * 128, :], ti, False)
        if stage >= 2:
            gla(128, ntri16, "ntri16", tri_b, "tri_b", prompt_state)
            P.op(ACT, lambda e: e.activation(out=S_b[:].rearrange("p h v -> p (h v)"),
                                             in_=S_f[:].rearrange("p h v -> p (h v)"), func=AF.Copy), ["S_f"], ["S_b"])
            fox_prompt(ti)
        if stage >= 3:
            k = ti % 2
            mem_attend_block(lambda h, mb: mkT[:, h, mb * 128:(mb + 1) * 128], "mkT",
                             lambda h, mb: mvA[:, mb, h, :], "mvA", PTm[k], "PT%d" % k, None, (0, 128), True, True)
            attn_finish(4, 128, omT, "omT")
        if stage >= 4:
            merge_out()
        if stage >= 5:
            peer(yp[ti * 128:(ti + 1) * 128, :])
    if stage >= 2:
        P.dma(SP, lambda e: e.dma_start(out=gsp.rearrange("h k v -> k h v"), in_=S_f[:]), ["S_f"], [ores()])
    if sample:
        sample_tile()

    if maxops is not None:
        for i, o in enumerate(P.ops[:maxops]):
            print(i, o.eng, "dma" if o.dma else "", o.reads, "->", o.writes)
        P.ops = P.ops[:maxops]
    P.op(SP, None, list(out_res), [])
    P.finalize(es)
    print("ops", len(P.ops), "max_sem", P.max_sem)
    return nc, es


def make_consts2():
    c = np.zeros((128, 760), np.float32)
    a = np.arange(128)[:, None]; b = np.arange(128)[None, :]
    c[:, 0:128] = ((a // 16) == (b // 16)) & (a > b)
    c[:, 128:256] = ((a // 8) == (b // 16))
    c[:, 256:384] = ((a // 8) == (8 + b // 16))
    c[:, 384:512] = ((a // 8) == (b // 8)) & (a > b)
    cc = np.arange(248)[None, :]
    c[:, 512:760] = (cc == 120 + a) & (a < 8)
    return c


def make_consts():
    c = np.zeros((128, 560), np.float32)
    j = np.arange(128)[:, None]; t = np.arange(128)[None, :]
    c[:, 0:128] = (j == t)
    c[:, 128:256] = (j <= t)
    c[:, 256:384] = (j <= t) & ((j // 8) == (t // 8))
    c[:, 384:512] = (j > t)
    c[:, 512:528] = np.arange(16)[None, :]
    c[:, 528:544] = ((np.arange(128)[:, None] // 8) == np.arange(16)[None, :])
    c[:, 544] = np.arange(128)
    return c


def run(inp, stage=99, ntiles=NT, sample=1, small_pool=False, compact=False, maxops=None):
    f = lambda a: np.ascontiguousarray(np.asarray(a))
    nphys = 4 if small_pool else (256 if compact else NPHYS)
    nexp = 128 if stage < 5 else 16384
    nc, es = build(stage=stage, nphys=nphys, ntiles=ntiles, sample=sample, maxops=maxops, nexp=nexp)
    cst = make_consts()
    if small_pool:
        ck = np.zeros((nphys * 128, 512), np.float32); cv = ck; clf = np.zeros((nphys * 128, 8), np.float32)
    elif compact:
        ck = cv = clf = None
    else:
        ck = f(inp["cache_fox_k"]).reshape(NPHYS * 128, 512)
        cv = f(inp["cache_fox_v"]).reshape(NPHYS * 128, 512)
        clf = f(inp["cache_fox_logf"]).reshape(NPHYS * 128, 8)
    shared = {
        "ck": ck, "cv": cv, "clf": clf,
        "g_mix": f(inp["g_mix"]).reshape(1, D), "w_in": f(inp["w_in"])[0], "w_a2": f(inp["w_a2"])[0],
        "b_a2": f(inp["b_a2"]).reshape(1, 512), "b_fgate": f(inp["b_fgate"]).reshape(1, 8),
        "b_gate": f(inp["b_gate"]).reshape(1, 3072), "g_gh": f(inp["g_gla_head"]).reshape(1, 128),
        "w_gla_o": f(inp["w_gla_o"])[0], "w_fox_o": f(inp["w_fox_o"])[0], "w_mem_o": f(inp["w_mem_o"])[0],
        "w_out": f(inp["w_out"])[0], "g_mem": f(inp["g_mem"]).reshape(1, D), "w_mem_kv": f(inp["w_mem_kv"])[0],
        "g_ffn": f(inp["g_ffn"]).reshape(1, D), "w_pq": f(inp["w_pq"])[0], "pk1": f(inp["peer_k1"])[0],
        "pk2": f(inp["peer_k2"])[0], "pu": f(inp["peer_u"])[0][:nexp], "pv": f(inp["peer_v"])[0][:nexp],
        "g_final": f(inp["g_final"]).reshape(1, D), "cst": cst, "cst2": make_consts2(),
    }
    in_maps = []
    for c in range(NCORES):
        m = dict(shared)
        m["xp"] = f(inp["x_prompt"][c])
        m["xs"] = f(inp["x_sample"][16 * c:16 * c + 16]).reshape(128, D)
        m["memp"] = f(inp["mem_prompt"][c])
        m["sg"] = f(inp["state_gla"][0, 16 * c:16 * c + 16]).reshape(64, 128, 128)
        m["cmk"] = f(inp["cache_mem_k"][0, 16 * c:16 * c + 16]).reshape(16, 256, 512)
        m["cmv"] = f(inp["cache_mem_v"][0, 16 * c:16 * c + 16]).reshape(16, 256, 512)
        m["pt"] = f(inp["page_table"][16 * c:16 * c + 16]).reshape(1, 256).astype(np.int32)
        if compact:
            ptc = m["pt"].reshape(256)
            perm = np.random.RandomState(c).permutation(256)
            inv = np.empty(256, np.int64); inv[perm] = np.arange(256)
            m["ck"] = f(inp["cache_fox_k"][0][ptc[perm]]).reshape(256 * 128, 512)
            m["cv"] = f(inp["cache_fox_v"][0][ptc[perm]]).reshape(256 * 128, 512)
            m["clf"] = f(inp["cache_fox_logf"][0][ptc[perm]]).reshape(256 * 128, 8)
            m["pt"] = inv.astype(np.int32).reshape(1, 256)
        in_maps.append(m)
    res = run_bass_kernel_spmd(nc, in_maps, core_ids=list(range(NCORES)))
    es.close()
    R = res.results
    cat = lambda k: np.stack([R[c][k] for c in range(NCORES)])
    y_prompt = cat("yp")
    y_sample = cat("ys").reshape(128, 8, D)
    fkp = cat("fkp").reshape(1, 8, SEQ, 8, 64)
    fvp = cat("fvp").reshape(1, 8, SEQ, 8, 64)
    lfp = cat("lfp").reshape(1, 8, SEQ, 8)
    gsp = cat("gsp").reshape(1, 8, 4, 128, 128)
    mkp = cat("mkp").reshape(1, 8, 256, 4, 128)
    mvp = cat("mvp").reshape(1, 8, 256, 4, 128)
    fks = cat("fks").reshape(1, 128, 8, 8, 64)
    fvs = cat("fvs").reshape(1, 128, 8, 8, 64)
    lfs = cat("lfs").reshape(1, 128, 8, 8)
    gss = cat("gss").reshape(1, 128, 4, 128, 128)
    return (y_prompt, y_sample, fkp, fvp, lfp, gsp, mkp, mvp, fks, fvs, lfs, gss)


def kernel(**inp):
    return run(inp)
```

```python
from contextlib import ExitStack
import numpy as np
import concourse.bass as bass
import concourse.mybir as mybir
from concourse.bass_utils import run_bass_kernel_spmd

F32 = mybir.dt.float32
BF16 = mybir.dt.bfloat16
U32 = mybir.dt.uint32
I32 = mybir.dt.int32
AF = mybir.ActivationFunctionType
ALU = mybir.AluOpType
AX = mybir.AxisListType

PE, ACT, DVE, POOL, SP = "tensor", "scalar", "vector", "gpsimd", "sync"
ENGS = [PE, ACT, DVE, POOL, SP]

NCORES = 8
D = 1024
SEQ = 2048
NT = SEQ // 128
EPS = 1e-6
IN_W = 7192
C_GQ, C_GK, C_GV, C_GR, C_GLR, C_FQ, C_FK, C_FV, C_FF, C_MQ, C_GT = (
    0, 512, 1024, 1536, 2048, 2064, 2576, 3088, 3600, 3608, 4120)
NPHYS = 2560
NEG = -1.0e30


class Op:
    __slots__ = ("eng", "fn", "reads", "writes", "dma", "deps", "sig", "tok", "idx", "grp", "slot", "clear")

    def __init__(self, eng, fn, reads, writes, dma, grp=None):
        self.eng = eng
        self.fn = fn
        self.reads = reads
        self.writes = writes
        self.dma = dma
        self.deps = []
        self.sig = False
        self.tok = None
        self.grp = grp
        self.slot = None


class Prog:
    N_DMA_SLOTS = 96

    def __init__(self, nc):
        self.nc = nc
        self.ops = []

    EXPAND = {"bank0": ("bank0h0", "bank0h1")}

    def _x(self, names):
        out = []
        for n in names:
            out.extend(self.EXPAND.get(n, (n,)))
        return tuple(out)

    def op(self, eng, fn, reads=(), writes=()):
        reads = self._x(reads); writes = self._x(writes)
        writes = tuple(writes) + tuple(r for r in reads if r.startswith("bank") and r not in writes)
        o = Op(eng, fn, reads, writes, False)
        self.ops.append(o)
        return o

    def dma(self, eng, fn, reads=(), writes=(), grp=None):
        o = Op(eng, fn, self._x(reads), self._x(writes), True, grp)
        self.ops.append(o)
        return o

    def finalize(self, es):
        nc = self.nc
        last_w = {}
        readers = {}
        for i, o in enumerate(self.ops):
            o.idx = i
            deps = set()
            for r in o.reads:
                w = last_w.get(r)
                if w is not None:
                    deps.add(w)
            for r in o.writes:
                w = last_w.get(r)
                if w is not None:
                    deps.add(w)
                for rd in readers.get(r, ()):
                    deps.add(rd)
            deps.discard(o)
            for r in o.reads:
                readers.setdefault(r, []).append(o)
            for r in o.writes:
                last_w[r] = o
                readers[r] = []
            dl = []
            for d in deps:
                if d.eng == PE and o.eng == PE and not d.dma and not o.dma:
                    continue
                d.sig = True
                dl.append(d)
            o.deps = dl
        esem = {e: es.enter_context(nc.semaphore("s_" + e)) for e in ENGS}
        NS = self.N_DMA_SLOTS
        NH = 24
        slots = [es.enter_context(nc.semaphore("d%d" % i)) for i in range(NS)]
        consumers = {}
        for o in self.ops:
            for d in o.deps:
                if d.dma:
                    consumers.setdefault(d, []).append(o)
        slot_val = [0] * NS
        slot_last = [None] * NS
        nh = nsw = 0
        for o in self.ops:
            if not o.dma:
                continue
            o.clear = False
            if o.eng == POOL:
                s = NH + nsw % (NS - NH)
                nsw += 1
                p = slot_last[s]
                if p is not None:
                    o.deps.append(p)
                slot_val[s] += 16
            else:
                s = nh % NH
                nh += 1
                p = slot_last[s]
                if p is not None:
                    o.deps.append(p)
                slot_val[s] += 16
            o.slot = s
            o.tok = (s, slot_val[s])
            slot_last[s] = o
        cnt = {e: 0 for e in ENGS}
        for o in self.ops:
            if not o.dma and o.sig:
                cnt[o.eng] += 1
                o.tok = (esem[o.eng], cnt[o.eng])
        self.max_sem = max(list(cnt.values()) + slot_val)

        def tok_of(d):
            if d.dma:
                return slots[d.tok[0]], d.tok[1]
            return d.tok

        per_eng = {e: [] for e in ENGS}
        for o in self.ops:
            per_eng[o.eng].append(o)

        def emit_all(e, ename):
            known = {}
            seen_sw = set()
            for o in per_eng[ename]:
                for d in o.deps:
                    sem, val = tok_of(d)
                    k = sem.num
                    if known.get(k, 0) < val:
                        e.wait_ge(sem, val)
                        known[k] = val
                if o.fn is None:
                    continue
                ins = o.fn(e)
                if o.dma:
                    ins.then_inc(slots[o.slot], 16)
                elif o.sig:
                    ins.then_inc(esem[ename], 1)

        block = es.enter_context(nc.Block())

        @block.tensor
        def _(e):
            emit_all(e, PE)

        @block.scalar
        def _(e):
            emit_all(e, ACT)

        @block.vector
        def _(e):
            emit_all(e, DVE)

        @block.gpsimd
        def _(e):
            emit_all(e, POOL)

        @block.sync
        def _(e):
            emit_all(e, SP)


def bc_rows(dram_ap_row, n):
    t = dram_ap_row
    return bass.AP(t.tensor, t.offset, [[0, 128], [1, n]])


def build(stage=99, nphys=NPHYS, ntiles=NT, sample=1, maxops=None, nexp=16384):
    nc = bass.Bass("TRN2", target_bir_lowering=False)

    def din(name, shape, dt=F32):
        return nc.dram_tensor(name, list(shape), dt, kind="ExternalInput").ap()

    def dout(name, shape, dt=F32):
        return nc.dram_tensor(name, list(shape), dt, kind="ExternalOutput").ap()

    xp = din("xp", [SEQ, D]); xs = din("xs", [128, D]); memp = din("memp", [256, D])
    ck = din("ck", [nphys * 128, 512]); cv = din("cv", [nphys * 128, 512]); clf = din("clf", [nphys * 128, 8])
    sg = din("sg", [64, 128, 128]); cmk = din("cmk", [16, 256, 512]); cmv = din("cmv", [16, 256, 512])
    pt = din("pt", [1, 256], I32)
    g_mix = din("g_mix", [1, D]); w_in = din("w_in", [D, IN_W]); w_a2 = din("w_a2", [16, 512])
    b_a2 = din("b_a2", [1, 512]); b_fgate = din("b_fgate", [1, 8]); b_gate = din("b_gate", [1, 3072])
    g_gh = din("g_gh", [1, 128]); w_gla_o = din("w_gla_o", [512, D]); w_fox_o = din("w_fox_o", [512, D])
    w_mem_o = din("w_mem_o", [512, D]); w_out = din("w_out", [D, D]); g_mem = din("g_mem", [1, D])
    w_mem_kv = din("w_mem_kv", [D, D]); g_ffn = din("g_ffn", [1, D]); w_pq = din("w_pq", [D, 2048])
    pk1 = din("pk1", [128, 128]); pk2 = din("pk2", [128, 128])
    pu = din("pu", [nexp, D]); pv = din("pv", [nexp, D]); g_final = din("g_final", [1, D])
    cst = din("cst", [128, 560])
    cst2 = din("cst2", [128, 760])

    yp = dout("yp", [SEQ, D]); ys = dout("ys", [128, D])
    fkp = dout("fkp", [SEQ, 512]); fvp = dout("fvp", [SEQ, 512]); lfp = dout("lfp", [SEQ, 8])
    gsp = dout("gsp", [4, 128, 128]); mkp = dout("mkp", [256, 512]); mvp = dout("mvp", [256, 512])
    fks = dout("fks", [128, 512]); fvs = dout("fvs", [128, 512]); lfs = dout("lfs", [128, 8])
    gss = dout("gss", [64, 128, 128])

    es = ExitStack()
    P = Prog(nc)
    uid = [0]

    sb_log = []

    def sb(shape, dt=F32, name=None):
        uid[0] += 1
        n = 1
        for d in shape[1:]:
            n *= d
        sb_log.append((name, n * (2 if dt == BF16 else 4)))
        try:
            return es.enter_context(nc.sbuf_tensor(name or ("t%d" % uid[0]), list(shape), dt))
        except AssertionError:
            print("SBUF:", sum(b for _, b in sb_log), sorted(sb_log, key=lambda x: -x[1])[:60])
            raise

    def pst(shape, dt=F32, name=None):
        uid[0] += 1
        return es.enter_context(nc.psum_tensor(name or ("p%d" % uid[0]), list(shape), dt))

    banks = [pst([128, 512], F32, "bank%d" % i) for i in range(8)]

    def bk(i):
        return banks[i]

    def bk_bf(i):
        return banks[i][:].bitcast(BF16)

    cst_f = sb([128, 560], F32, "cst_f")
    P.dma(SP, lambda e: e.dma_start(out=cst_f[:], in_=cst[:, :]), [], ["cst_f"])
    cst2_f = sb([128, 760], F32, "cst2_f")
    P.dma(SP, lambda e: e.dma_start(out=cst2_f[:], in_=cst2[:, :]), [], ["cst2_f"])
    Mst_f = cst2_f[:, 0:128]
    Bsel_f = [cst2_f[:, 128:256], cst2_f[:, 256:384]]
    bustr8_f = cst2_f[:, 384:512]
    Esel_f = cst2_f[:, 512:760]
    bmask = cst_f[:, 528:544]
    ident_f = cst_f[:, 0:128]
    tri_f = cst_f[:, 128:256]
    btri_f = cst_f[:, 256:384]
    ustr_f = cst_f[:, 384:512]
    iota16_f = cst_f[:, 512:528]
    ident_b = sb([128, 128], BF16, "ident_b")
    ones_b = sb([128, 128], BF16, "ones_b")
    ones_f = sb([128, 128], F32, "ones_f")
    tri_b = sb([128, 128], BF16, "tri_b")
    btri_b = sb([128, 128], BF16, "btri_b")
    ntri16 = sb([128, 128], F32, "ntri16")
    nbtri16 = sb([128, 128], F32, "nbtri16")
    P.op(DVE, lambda e: e.tensor_copy(out=ident_b[:], in_=ident_f), ["cst_f"], ["ident_b"])
    P.op(DVE, lambda e: e.memset(ones_b[:], 1.0), [], ["ones_b"])
    P.op(DVE, lambda e: e.memset(ones_f[:], 1.0), [], ["ones_f"])
    P.op(DVE, lambda e: e.tensor_copy(out=tri_b[:], in_=tri_f), ["cst_f"], ["tri_b"])
    P.op(DVE, lambda e: e.tensor_copy(out=btri_b[:], in_=btri_f), ["cst_f"], ["btri_b"])
    P.op(DVE, lambda e: e.tensor_scalar(out=ntri16[:], in0=tri_f, scalar1=-1.0 / 16.0, scalar2=None, op0=ALU.mult),
         ["cst_f"], ["ntri16"])
    P.op(DVE, lambda e: e.tensor_scalar(out=nbtri16[:], in0=btri_f, scalar1=-1.0 / 16.0, scalar2=None, op0=ALU.mult),
         ["cst_f"], ["nbtri16"])

    gmix_bc = sb([128, D], F32, "gmix_bc"); gffn_bc = sb([128, D], F32, "gffn_bc")
    gfin_bc = sb([128, D], F32, "gfin_bc"); acc = sb([128, D], F32, "acc")
    gmem_bc = acc
    for tl, src, nm in ((gmix_bc, g_mix, "gmix_bc"), (gffn_bc, g_ffn, "gffn_bc"), (gfin_bc, g_final, "gfin_bc"),
                        (gmem_bc, g_mem, "acc")):
        P.dma(SP, (lambda tl, src: lambda e: e.dma_start(out=tl[:], in_=bc_rows(src, D)))(tl, src), [], [nm])
    bfg_bc = sb([128, 8], F32, "bfg_bc")
    P.dma(SP, lambda e: e.dma_start(out=bfg_bc[:], in_=bc_rows(b_fgate, 8)), [], ["bfg_bc"])
    bg_all = sb([128, 25], F32, "bgate_c")
    bgate_c = bg_all[:, 0:24]
    ggh_c = bg_all[:, 24:25]
    stg = sb([25, 128], F32, "stg")
    P.dma(SP, lambda e: e.dma_start(out=stg[0:24, :], in_=b_gate.rearrange("o (c p) -> (o c) p", p=128)), [], ["stg"])
    P.dma(SP, lambda e: e.dma_start(out=stg[24:25, :], in_=g_gh[:, :]), [], ["stg"])
    P.op(PE, lambda e: e.transpose(out=banks[0][:, 0:25], in_=stg[0:25, :], identity=ident_f[0:25, 0:25]),
         ["stg", "cst_f"], ["bank0"])
    P.op(ACT, lambda e: e.activation(out=bg_all[:], in_=banks[0][:, 0:25], func=AF.Copy), ["bank0"], ["bgate_c"])
    wa2_f = sb([17, 512], F32, "wa2_f")
    P.dma(SP, lambda e: e.dma_start(out=wa2_f[0:16, :], in_=w_a2[:, :]), [], ["wa2_f"])
    P.dma(SP, lambda e: e.dma_start(out=wa2_f[16:17, :], in_=b_a2[:, :]), [], ["wa2_f"])

    NRING = 2
    ring = [sb([128, 4096], BF16, "ring%d" % i) for i in range(NRING)]
    ring_i = [0]

    def wload(dram_view, shape3):
        i = ring_i[0] % NRING
        ring_i[0] += 1
        a, b = shape3
        t = ring[i]
        dst = t[:, 0:a * b].rearrange("p (a b) -> p a b", a=a)
        nm = "ring%d" % i
        P.dma(POOL, lambda e: e.dma_start(out=dst, in_=dram_view), [], [nm])
        return dst, nm

    w_in_v = w_in.rearrange("(c p) n -> p c n", p=128)

    PS = 512
    x_t = sb([128, D], F32, "x_t"); junk_b = sb([128, D], BF16, "junk_b")
    ss = sb([128, 1], F32, "ss"); rstd = sb([128, 1], F32, "rstd")
    n_b = sb([128, D], BF16, "n_b"); nT = sb([128, 8, 128], BF16, "nT")
    fk_tm = sb([128, 512], F32, "fk_tm"); fv_tm = sb([128, 512], F32, "fv_tm")
    KT = sb([128, 4, SEQ], BF16, "KT")
    VaugR = sb([128, NT * 8 * 66], BF16, "VaugR")
    Vaug = VaugR[:].rearrange("p (a h d) -> p a h d", a=NT, h=8)
    VAUG_ALL = ["Vaug%d" % i for i in range(NT)]
    lf_t = sb([128, 8], F32, "lf_t"); lf_e = sb([128, 8], F32, "lf_e")
    qT_f = sb([128, 4, 128], F32, "qT_f"); kT_f = sb([128, 4, 128], F32, "kT_f")
    v_b = sb([128, 512], BF16, "v_b"); rT = sb([128, 4, 128], BF16, "rT")
    glrT = sb([17, 128], F32, "glrT")
    P.op(DVE, lambda e: e.memset(glrT[:], 1.0), [], ["glrT"])
    qfT = sb([128, 4, 128], BF16, "qfT"); qmT = sb([128, 4, 128], BF16, "qmT")
    gatesT = sb([128, 24, 128], BF16, "gatesT")
    for q in range(4):
        P.op(DVE, (lambda q: lambda e: e.memset(VaugR[:, q * 2112:(q + 1) * 2112], 1.0))(q), [], VAUG_ALL)

    def pstride(t):
        return t[:].ap[0][0]

    def AP(t, off, dims):
        return bass.AP(t[:].tensor, off, [[pstride(t), 128]] + [list(d) for d in dims])

    bank_rr = [0]

    def nbank():
        b = 1 + bank_rr[0] % 3
        bank_rr[0] += 1
        return b

    def rmsnorm_rows(src, src_res, g_bc, g_res, dst, dst_res):
        P.op(ACT, lambda e: e.activation(out=junk_b[:], in_=src, func=AF.Square, accum_out=ss[:]),
             [src_res], ["junk_b", "ss"])
        P.op(DVE, lambda e: e.tensor_scalar(out=rstd[:], in0=ss[:], scalar1=1.0 / D, scalar2=EPS,
                                            op0=ALU.mult, op1=ALU.add), ["ss"], ["rstd"])
        P.op(ACT, lambda e: e.activation(out=rstd[:], in_=rstd[:], func=AF.Ln), ["rstd"], ["rstd"])
        P.op(ACT, lambda e: e.activation(out=rstd[:], in_=rstd[:], func=AF.Exp, scale=-0.5), ["rstd"], ["rstd"])
        P.op(DVE, lambda e: e.scalar_tensor_tensor(out=dst, in0=src, scalar=rstd[:], in1=g_bc[:],
                                                   op0=ALU.mult, op1=ALU.mult),
             [src_res, "rstd", g_res], [dst_res])

    def transpose_rows(src_b, src_res, dstT, dst_res, nchunk=8):
        pb = bk_bf(0)
        for c in range(nchunk):
            P.op(PE, (lambda c: lambda e: e.transpose(out=pb[:, c * 128:(c + 1) * 128],
                                                       in_=src_b[:, c * 128:(c + 1) * 128], identity=ident_b[:]))(c),
                 [src_res, "ident_b"], ["bank0"])
        P.op(ACT, lambda e: e.activation(out=dstT[:].rearrange("p c t -> p (c t)"), in_=pb[:, 0:nchunk * 128],
                                         func=AF.Copy), ["bank0"], [dst_res])

    def proj_tm(wview, ncols, evac, src=None, src_res="nT"):
        src = nT if src is None else src
        wv, wn = wload(wview, (8, ncols))
        b = nbank()
        pb = bk(b)
        for c in range(8):
            P.op(PE, (lambda c: lambda e: e.matmul(pb[:, 0:ncols], lhsT=src[:, c, :], rhs=wv[:, c, :],
                                                    start=(c == 0), stop=(c == 7)))(c),
                 [src_res, wn], ["bank%d" % b])
        evac(pb[:, 0:ncols], "bank%d" % b)

    def proj_fm(wview, ncols, evac, src=None, src_res="nT"):
        src = nT if src is None else src
        wv, wn = wload(wview, (8, ncols))
        b = nbank()
        pb = bk(b)
        nch = (ncols + 127) // 128
        for j in range(nch):
            w = min(128, ncols - j * 128)
            for c in range(8):
                P.op(PE, (lambda c, j, w: lambda e: e.matmul(pb[0:w, j * 128:(j + 1) * 128],
                                                             lhsT=wv[:, c, j * 128:j * 128 + w], rhs=src[:, c, :],
                                                             start=(c == 0), stop=(c == 7)))(c, j, w),
                     [src_res, wn], ["bank%d" % b])
        evac(pb[:, 0:512], "bank%d" % b)

    def win(c0, n):
        return w_in_v[:, :, c0:c0 + n]

    out_res = []

    def ores():
        nm = "out%d" % len(out_res)
        out_res.append(nm)
        return nm

    KTn = sb([128, 4, 128], BF16, "KTn"); Vn = sb([128, 8, 66], BF16, "Vn")
    P.op(DVE, lambda e: e.memset(Vn[:].rearrange("p h d -> p (h d)"), 1.0), [], ["Vn"])

    def front(x_src, ti, is_sample):
        P.dma(SP, lambda e: e.dma_start(out=x_t[:], in_=x_src), [], ["x_t"])
        rmsnorm_rows(x_t[:], "x_t", gmix_bc, "gmix_bc", n_b[:], "n_b")
        transpose_rows(n_b, "n_b", nT, "nT")
        fk_out = (fks[:, :] if is_sample else fkp[ti * 128:(ti + 1) * 128, :])
        fv_out = (fvs[:, :] if is_sample else fvp[ti * 128:(ti + 1) * 128, :])
        lf_out = (lfs[:, :] if is_sample else lfp[ti * 128:(ti + 1) * 128, :])

        def ev_fk(ps, res):
            P.op(ACT, lambda e: e.activation(out=fk_tm[:], in_=ps, func=AF.Copy), [res], ["fk_tm"])
            P.dma(SP, lambda e: e.dma_start(out=fk_out, in_=fk_tm[:]), ["fk_tm"], [ores()])
        proj_tm(win(C_FK, 512), 512, ev_fk)

        def ev_fv(ps, res):
            P.op(ACT, lambda e: e.activation(out=fv_tm[:], in_=ps, func=AF.Copy), [res], ["fv_tm"])
            if is_sample:
                P.op(DVE, lambda e: e.tensor_copy(out=Vn[:, :, 0:64],
                                                  in_=fv_tm[:].rearrange("p (h d) -> p h d", h=8)), ["fv_tm"], ["Vn"])
            else:
                P.op(DVE, lambda e: e.tensor_copy(out=Vaug[:, ti, :, 0:64],
                                                  in_=fv_tm[:].rearrange("p (h d) -> p h d", h=8)), ["fv_tm"], ["Vaug%d" % ti])
            P.dma(SP, lambda e: e.dma_start(out=fv_out, in_=fv_tm[:]), ["fv_tm"], [ores()])
        proj_tm(win(C_FV, 512), 512, ev_fv)

        def ev_ff(ps, res):
            P.op(DVE, lambda e: e.tensor_tensor(out=lf_e[:], in0=ps, in1=bfg_bc[:], op=ALU.add),
                 [res, "bfg_bc"], ["lf_e"])
            P.op(ACT, lambda e: e.activation(out=lf_e[:], in_=lf_e[:], func=AF.Exp, scale=-1.0), ["lf_e"], ["lf_e"])
            P.op(ACT, lambda e: e.activation(out=lf_e[:], in_=lf_e[:], func=AF.Ln, bias=1.0), ["lf_e"], ["lf_e"])
            P.op(DVE, lambda e: e.tensor_scalar(out=lf_t[:], in0=lf_e[:], scalar1=-1.0, scalar2=None, op0=ALU.mult),
                 ["lf_e"], ["lf_t"])
            P.dma(SP, lambda e: e.dma_start(out=lf_out, in_=lf_t[:]), ["lf_t"], [ores()])
        proj_tm(win(C_FF, 8), 8, ev_ff)

        def cp(dst, res_dst, func=AF.Copy):
            def ev(ps, res):
                P.op(ACT, lambda e: e.activation(out=dst, in_=ps, func=func), [res], [res_dst])
            return ev
        proj_fm(win(C_GQ, 512), 512, cp(qT_f[:].rearrange("p c t -> p (c t)"), "qT_f"))
        proj_fm(win(C_GK, 512), 512, cp(kT_f[:].rearrange("p c t -> p (c t)"), "kT_f"))
        proj_tm(win(C_GV, 512), 512, cp(v_b[:], "v_b"))
        proj_fm(win(C_GR, 512), 512, cp(rT[:].rearrange("p c t -> p (c t)"), "rT", AF.Silu))

        def ev_glr(ps, res):
            P.op(ACT, lambda e: e.activation(out=glrT[0:16, :], in_=ps[0:16, 0:128], func=AF.Copy), [res], ["glrT"])
        proj_fm(win(C_GLR, 16), 16, ev_glr)
        proj_fm(win(C_FQ, 512), 512, cp(qfT[:].rearrange("p c t -> p (c t)"), "qfT"))

        def ev_kT(ps, res):
            if is_sample:
                P.op(ACT, lambda e: e.activation(out=KTn[:], in_=ps.rearrange("p (c t) -> p c t", c=4), func=AF.Copy),
                     [res], ["KTn"])
            else:
                P.op(ACT, lambda e: e.activation(out=KT[:, :, ti * 128:(ti + 1) * 128],
                                                 in_=ps.rearrange("p (c t) -> p c t", c=4), func=AF.Copy),
                     [res], ["KT%d" % ti])
        proj_fm(win(C_FK, 512), 512, ev_kT)
        proj_fm(win(C_MQ, 512), 512, cp(qmT[:].rearrange("p c t -> p (c t)"), "qmT"))
        for gb in range(6):
            def ev_g(ps, res, gb=gb):
                for j in range(4):
                    ch = gb * 4 + j
                    P.op(ACT, (lambda j, ch: lambda e: e.activation(out=gatesT[:, ch, :],
                                                                    in_=ps[:, j * 128:(j + 1) * 128],
                                                                    func=AF.Sigmoid, bias=bg_all[:, ch:ch + 1]))(j, ch),
                         [res, "bgate_c"], ["gatesT%d" % ch])
            proj_fm(win(C_GT + gb * 512, 512), 512, ev_g)

    sp_f = sb([128, 512], F32, "sp_f")
    Epos = sb([128, 4, 128], F32, "Epos"); Eneg = sb([128, 4, 128], F32, "Eneg")
    qtl = sb([128, 4, 128], BF16, "qtl"); ktl = sb([128, 4, 128], BF16, "ktl"); khT = sb([128, 4, 128], BF16, "khT")
    kh = sb([128, 512], BF16, "kh"); attT = sb([128, 4, 128], BF16, "attT")
    S_f = sb([128, 4, 128], F32, "S_f"); S_b = sb([128, 4, 128], BF16, "S_b")
    o_sb = sb([128, 512], F32, "o_sb"); sq_b = sb([128, 512], BF16, "sq_b")
    sd_f = sb([128, 512], F32, "sd_f"); rs_f = sd_f; t1_f = o_sb
    ogT = sb([128, 4, 128], BF16, "ogT")
    vm_b = [sb([128, 512], BF16, "vm_b%d" % i) for i in range(2)]
    P.op(DVE, lambda e: e.memset(S_f[:].rearrange("p h v -> p (h v)"), 0.0), [], ["S_f"])
    P.op(DVE, lambda e: e.memset(S_b[:].rearrange("p h v -> p (h v)"), 0.0), [], ["S_b"])

    def gla(L, nmask16, nmask_res, mask_b, mask_res, seq_states):
        nseq = 128 // L
        b1 = nbank()
        P.op(PE, lambda e: e.matmul(bk(b1)[:, :], lhsT=glrT[:, :], rhs=wa2_f[:, :], start=True, stop=True),
             ["glrT", "wa2_f"], ["bank%d" % b1])
        P.op(ACT, lambda e: e.activation(out=sp_f[:], in_=bk(b1)[:, :], func=AF.Exp, scale=-1.0),
             ["bank%d" % b1], ["sp_f"])
        P.op(ACT, lambda e: e.activation(out=sp_f[:], in_=sp_f[:], func=AF.Ln, bias=1.0), ["sp_f"], ["sp_f"])
        b2 = nbank()
        for h in range(4):
            P.op(PE, (lambda h: lambda e: e.matmul(bk(b2)[:, h * 128:(h + 1) * 128], lhsT=sp_f[:, h * 128:(h + 1) * 128],
                                                    rhs=nmask16[:], start=True, stop=True))(h),
                 ["sp_f", nmask_res], ["bank%d" % b2])
        fl = lambda t: t[:].rearrange("p c t -> p (c t)")
        P.op(ACT, lambda e: e.activation(out=fl(Epos), in_=bk(b2)[:, :], func=AF.Exp), ["bank%d" % b2], ["Epos"])
        P.op(ACT, lambda e: e.activation(out=fl(Eneg), in_=bk(b2)[:, :], func=AF.Exp, scale=-1.0),
             ["bank%d" % b2], ["Eneg"])
        P.op(DVE, lambda e: e.scalar_tensor_tensor(out=fl(qtl), in0=fl(qT_f), scalar=128.0 ** -0.5, in1=fl(Epos),
                                                   op0=ALU.mult, op1=ALU.mult), ["qT_f", "Epos"], ["qtl"])
        P.op(DVE, lambda e: e.tensor_tensor(out=fl(ktl), in0=fl(kT_f), in1=fl(Eneg), op=ALU.mult),
             ["kT_f", "Eneg"], ["ktl"])
        P.op(DVE, lambda e: e.tensor_tensor(
            out=AP(khT, 0, [[128, 4], [L, nseq], [1, L]]), in0=AP(ktl, 0, [[128, 4], [L, nseq], [1, L]]),
            in1=AP(Epos, L - 1, [[128, 4], [L, nseq], [0, L]]), op=ALU.mult), ["ktl", "Epos"], ["khT"])
        pb0 = bk_bf(0)
        for h in range(4):
            P.op(PE, (lambda h: lambda e: e.transpose(out=pb0[:, h * 128:(h + 1) * 128], in_=khT[:, h, :],
                                                       identity=ident_b[:]))(h), ["khT", "ident_b"], ["bank0"])
        P.op(ACT, lambda e: e.activation(out=kh[:], in_=pb0[:, 0:512], func=AF.Copy), ["bank0"], ["kh"])
        b3 = nbank()
        for h in range(4):
            P.op(PE, (lambda h: lambda e: e.matmul(bk(b3)[:, h * 128:(h + 1) * 128], lhsT=ktl[:, h, :], rhs=qtl[:, h, :],
                                                    start=True, stop=True))(h), ["ktl", "qtl"], ["bank%d" % b3])
        P.op(DVE, lambda e: e.tensor_tensor(out=attT[:], in0=bk(b3)[:, :].rearrange("p (h t) -> p h t", h=4),
                                            in1=AP(mask_b, 0, [[0, 4], [1, 128]]), op=ALU.mult),
             ["bank%d" % b3, mask_res], ["attT"])
        b4 = nbank()
        for h in range(4):
            P.op(PE, (lambda h: lambda e: e.matmul(bk(b4)[:, h * 128:(h + 1) * 128], lhsT=v_b[:, h * 128:(h + 1) * 128],
                                                    rhs=attT[:, h, :], start=(h == 0), stop=False,
                                                    skip_group_check=True))(h),
                 ["v_b", "attT"], ["bank%d" % b4])
        for b in range(nseq):
            st = seq_states[b]
            if st.get("pre_o"):
                st["pre_o"]()
            Sb, rb = st["Sb"], st["rb"]
            for h in range(4):
                P.op(PE, (lambda h, b, Sb: lambda e: e.matmul(
                    bk(b4)[:, h * 128 + b * L:h * 128 + (b + 1) * L], lhsT=Sb(h), rhs=qtl[:, h, b * L:(b + 1) * L],
                    start=False, stop=(b == nseq - 1), skip_group_check=True))(h, b, Sb), [rb, "qtl"], ["bank%d" % b4])
        P.op(ACT, lambda e: e.activation(out=o_sb[:], in_=bk(b4)[:, :], func=AF.Copy), ["bank%d" % b4], ["o_sb"])
        P.op(ACT, lambda e: e.activation(out=sq_b[:], in_=bk(b4)[:, :], func=AF.Square), ["bank%d" % b4], ["sq_b"])
        b5 = nbank()
        P.op(PE, lambda e: e.matmul(bk(b5)[:, :], lhsT=ones_b[:], rhs=sq_b[:], start=True, stop=True),
             ["ones_b", "sq_b"], ["bank%d" % b5])
        P.op(DVE, lambda e: e.tensor_scalar(out=sd_f[:], in0=bk(b5)[:, :], scalar1=1.0 / 128.0, scalar2=EPS,
                                            op0=ALU.mult, op1=ALU.add), ["bank%d" % b5], ["sd_f"])
        P.op(ACT, lambda e: e.activation(out=sd_f[:], in_=sd_f[:], func=AF.Ln), ["sd_f"], ["sd_f"])
        P.op(ACT, lambda e: e.activation(out=sd_f[:], in_=sd_f[:], func=AF.Exp, scale=-0.5), ["sd_f"], ["sd_f"])
        P.op(DVE, lambda e: e.scalar_tensor_tensor(out=t1_f[:], in0=o_sb[:], scalar=ggh_c, in1=rs_f[:],
                                                   op0=ALU.mult, op1=ALU.mult), ["o_sb", "bgate_c", "sd_f"], ["o_sb"])
        P.op(DVE, lambda e: e.tensor_tensor(out=fl(ogT), in0=t1_f[:], in1=fl(rT), op=ALU.mult),
             ["o_sb", "rT"], ["ogT"])
        for b in range(nseq):
            st = seq_states[b]
            Sf, rf, use_mask = st["Sf"], st["rf"], st["use_mask"]
            if st.get("pre_s"):
                st["pre_s"]()
            if use_mask:
                vm = vm_b[b % 2]
                vres = "vm_b%d" % (b % 2)
                P.op(DVE, (lambda b, vm: lambda e: e.tensor_scalar(out=vm[:], in0=v_b[:], scalar1=bmask[:, b:b + 1],
                                                                   scalar2=None, op0=ALU.mult))(b, vm),
                     ["v_b", "cst_f"], [vres])
            else:
                vm = v_b
                vres = "v_b"
            b6 = nbank()
            for h in range(4):
                P.op(PE, (lambda h, vm, b6: lambda e: e.matmul(bk(b6)[:, h * 128:(h + 1) * 128],
                                                               lhsT=kh[:, h * 128:(h + 1) * 128],
                                                               rhs=vm[:, h * 128:(h + 1) * 128], start=True, stop=True))(h, vm, b6),
                     ["kh", vres], ["bank%d" % b6])
            for h in range(4):
                P.op(DVE, (lambda h, b, Sf, b6: lambda e: e.scalar_tensor_tensor(
                    out=Sf(h), in0=Sf(h), scalar=Epos[:, h, b * L + L - 1:b * L + L], in1=bk(b6)[:, h * 128:(h + 1) * 128],
                    op0=ALU.mult, op1=ALU.add))(h, b, Sf, b6), [rf, "Epos", "bank%d" % b6], [rf])
            if st.get("post_s"):
                st["post_s"]()

    negd = sb([128, NT, 8], F32, "negd"); Rsum = sb([128, 8], F32, "Rsum"); dend = sb([128, 8], F32, "dend")
    biasb = [sb([128, 8], F32, "biasb%d" % i) for i in range(2)]
    PT = [sb([128, 8, 128], BF16, "PT%d" % i) for i in range(2)]
    rinv = sb([128, 8], F32, "rinv")
    o_n = sb([128, 512], BF16, "o_n"); ofT = sb([128, 4, 128], BF16, "ofT"); omT = sb([128, 4, 128], BF16, "omT")
    P.op(DVE, lambda e: e.memset(Rsum[:], 0.0), [], ["Rsum"])
    pt_i = [0]
    qbd = sb([128, 8, 128], BF16, "qbd")
    P.op(DVE, lambda e: e.memset(qbd[:].rearrange("p h t -> p (h t)"), 0.0), [], ["qbd"])

    def build_qbd():
        qv = qbd[:].rearrange("p (c r) t -> p c r t", r=2)
        P.op(DVE, lambda e: e.tensor_copy(out=qv[0:64, :, 0, :], in_=qfT[0:64, :, :]), ["qfT"], ["qbd"])
        P.op(DVE, lambda e: e.tensor_copy(out=qv[64:128, :, 1, :], in_=qfT[64:128, :, :]), ["qfT"], ["qbd"])

    def fox_block(kt_ap_fn, kt_res, v_ap_fn, v_res, bias_ap, bias_res, mask_b, mask_res, first, last):
        k = pt_i[0] % 2
        pt_i[0] += 1
        ptb = PT[k]
        pres = "PT%d" % k
        for h in range(8):
            c = h // 2
            bkk = 4 + h // 4
            P.op(PE, (lambda h, c, bkk: lambda e: e.matmul(bk(bkk)[:, (h % 4) * 128:(h % 4 + 1) * 128],
                                                           lhsT=kt_ap_fn(c), rhs=qbd[:, h, :],
                                                           start=True, stop=True))(h, c, bkk),
                 [kt_res, "qbd"], ["bank%d" % bkk])
        for h in range(8):
            bkk = 4 + h // 4
            P.op(ACT, (lambda h, bkk: lambda e: e.activation(out=ptb[:, h, :], in_=bk(bkk)[:, (h % 4) * 128:(h % 4 + 1) * 128],
                                                             func=AF.Exp, scale=0.125, bias=bias_ap[:, h:h + 1]))(h, bkk),
                 ["bank%d" % bkk, bias_res], [pres])
        if mask_b is not None:
            P.op(DVE, lambda e: e.tensor_tensor(out=ptb[:], in0=ptb[:], in1=AP(mask_b, 0, [[0, 8], [1, 128]]),
                                                op=ALU.mult), [pres, mask_res], [pres])
        for h in range(8):
            bkk = 6 + h // 4
            P.op(PE, (lambda h, bkk: lambda e: e.matmul(bk(bkk)[:, (h % 4) * 66:(h % 4) * 66 + 66], lhsT=ptb[:, h, :],
                                                        rhs=v_ap_fn(h), start=(first and h % 4 == 0),
                                                        stop=last, skip_group_check=True))(h, bkk),
                 [pres, v_res], ["bank%d" % bkk])

    def attn_finish(nh, dh, dstT, dst_res):
        hp = nh // 2
        w = dh + 2
        for half in range(2):
            bkk = 6 + half
            v3 = bk(bkk)[:, 0:hp * w].rearrange("p (h d) -> p h d", d=w)
            P.op(DVE, (lambda v3, half: lambda e: e.reciprocal(out=rinv[:, half * hp:(half + 1) * hp],
                                                               in_=v3[:, :, dh]))(v3, half),
                 ["bank%d" % bkk], ["rinv"])
            P.op(DVE, (lambda v3, half: lambda e: e.tensor_tensor(
                out=o_n[:, half * 256:(half + 1) * 256].rearrange("p (h d) -> p h d", d=dh), in0=v3[:, :, 0:dh],
                in1=AP(rinv, half * hp, [[1, hp], [0, dh]]), op=ALU.mult))(v3, half),
                ["bank%d" % bkk, "rinv"], ["o_n"])
        transpose_rows(o_n, "o_n", dstT, dst_res, nchunk=4)

    def fox_prompt(i):
        build_qbd()
        b1 = nbank()
        P.op(PE, lambda e: e.matmul(bk(b1)[:, 0:8], lhsT=tri_f, rhs=lf_t[:], start=True, stop=False),
             ["cst_f", "lf_t"], ["bank%d" % b1])
        P.op(PE, lambda e: e.matmul(bk(b1)[:, 0:8], lhsT=ones_f[:], rhs=Rsum[:], start=False, stop=True),
             ["ones_f", "Rsum"], ["bank%d" % b1])
        P.op(DVE, lambda e: e.tensor_scalar(out=negd[:, i, :], in0=bk(b1)[:, 0:8], scalar1=-1.0, scalar2=None,
                                            op0=ALU.mult), ["bank%d" % b1], ["negd%d" % i])
        P.op(DVE, lambda e: e.tensor_tensor(out=Rsum[:], in0=Rsum[:], in1=lf_t[:], op=ALU.add),
             ["Rsum", "lf_t"], ["Rsum"])
        b2 = nbank()
        P.op(PE, lambda e: e.matmul(bk(b2)[:, 0:8], lhsT=ones_f[:], rhs=Rsum[:], start=True, stop=True),
             ["ones_f", "Rsum"], ["bank%d" % b2])
        P.op(DVE, lambda e: e.tensor_copy(out=dend[:], in_=bk(b2)[:, 0:8]), ["bank%d" % b2], ["dend"])
        for j in range(i + 1):
            bb = biasb[j % 2]
            bres = "biasb%d" % (j % 2)
            P.op(DVE, (lambda j, bb: lambda e: e.tensor_tensor(out=bb[:], in0=dend[:], in1=negd[:, j, :], op=ALU.add))(j, bb),
                 ["dend", "negd%d" % j], [bres])
            fox_block(lambda c, j=j: KT[:, c, j * 128:(j + 1) * 128], "KT%d" % j,
                      lambda h, j=j: Vaug[:, j, h, :], "Vaug%d" % j, bb, bres,
                      tri_b if j == i else None, "tri_b", j == 0, j == i)
        attn_finish(8, 64, ofT, "ofT")

    mkT = sb([128, 4, 256], BF16, "mkT"); mvA = sb([128, 2, 4, 130], BF16, "mvA")
    PTm = PT
    mk_tm = fk_tm; mv_tm = fv_tm
    mnT = nT
    P.op(DVE, lambda e: e.memset(mvA[:].rearrange("p a h d -> p (a h d)"), 1.0), [], ["mvA"])
    w_mkv_v = w_mem_kv.rearrange("(c p) n -> p c n", p=128)

    def mem_setup_prompt():
        for mb in range(2):
            P.dma(SP, (lambda mb: lambda e: e.dma_start(out=x_t[:], in_=memp[mb * 128:(mb + 1) * 128, :]))(mb), [], ["x_t"])
            rmsnorm_rows(x_t[:], "x_t", gmem_bc, "acc", n_b[:], "n_b")
            transpose_rows(n_b, "n_b", mnT, "nT")

            def ev_k(ps, res, mb=mb):
                P.op(ACT, lambda e: e.activation(out=mk_tm[:], in_=ps, func=AF.Copy), [res], ["fk_tm"])
                P.dma(SP, lambda e: e.dma_start(out=mkp[mb * 128:(mb + 1) * 128, :], in_=mk_tm[:]), ["fk_tm"], [ores()])
            proj_tm(w_mkv_v[:, :, 0:512], 512, ev_k, mnT, "nT")

            def ev_v(ps, res, mb=mb):
                P.op(ACT, lambda e: e.activation(out=mv_tm[:], in_=ps, func=AF.Copy), [res], ["fv_tm"])
                P.op(DVE, lambda e: e.tensor_copy(out=mvA[:, mb, :, 0:128], in_=mv_tm[:].rearrange("p (h d) -> p h d", h=4)),
                     ["fv_tm"], ["mvA"])
                P.dma(SP, lambda e: e.dma_start(out=mvp[mb * 128:(mb + 1) * 128, :], in_=mv_tm[:]), ["fv_tm"], [ores()])
            proj_tm(w_mkv_v[:, :, 512:1024], 512, ev_v, mnT, "nT")

            def ev_kT(ps, res, mb=mb):
                P.op(ACT, lambda e: e.activation(out=mkT[:, :, mb * 128:(mb + 1) * 128],
                                                 in_=ps.rearrange("p (c t) -> p c t", c=4), func=AF.Copy), [res], ["mkT"])
            proj_fm(w_mkv_v[:, :, 0:512], 512, ev_kT, mnT, "nT")

    def mem_attend_block(mkT_fn, mk_res, mv_fn, mv_res, ptm, ptm_res, out_ap_fn, cols, first, last):
        c0, n = cols
        for h in range(4):
            for mb in range(2):
                idx = h * 2 + mb
                bkk = 4 + idx // 4
                P.op(PE, (lambda h, mb, idx, bkk: lambda e: e.matmul(bk(bkk)[:, (idx % 4) * 128:(idx % 4) * 128 + n],
                                                                      lhsT=mkT_fn(h, mb), rhs=qmT[:, h, c0:c0 + n],
                                                                      start=True, stop=True))(h, mb, idx, bkk),
                     [mk_res, "qmT"], ["bank%d" % bkk])
        for half in range(2):
            bkk = 4 + half
            P.op(ACT, (lambda half, bkk: lambda e: e.activation(
                out=ptm[:, half * 4:(half + 1) * 4, c0:c0 + n],
                in_=bk(bkk)[:, :].rearrange("p (i t) -> p i t", i=4)[:, :, 0:n], func=AF.Exp, scale=128.0 ** -0.5))(half, bkk),
                ["bank%d" % bkk], [ptm_res])
        for h in range(4):
            for mb in range(2):
                bkk = 6 + h // 2
                P.op(PE, (lambda h, mb, bkk: lambda e: e.matmul(bk(bkk)[:, (h % 2) * 130:(h % 2) * 130 + 130],
                                                                lhsT=ptm[:, h * 2 + mb, :], rhs=mv_fn(h, mb),
                                                                start=(first and h % 2 == 0 and mb == 0),
                                                                stop=(last and mb == 1), skip_group_check=True))(h, mb, bkk),
                     [ptm_res, mv_res], ["bank%d" % bkk])

    m_acc = sb([128, 512], F32, "m_acc"); m_tmp = sb([128, 512], F32, "m_tmp")
    mT = sb([128, 8, 128], BF16, "mT"); h_t = x_t
    wo_views = [w.rearrange("(c p) n -> p c n", p=128) for w in (w_gla_o, w_fox_o, w_mem_o)]
    w_out_v = w_out.rearrange("(c p) n -> p c n", p=128)
    brT = [(ogT, "ogT"), (ofT, "ofT"), (omT, "omT")]

    def merge_out():
        for half in range(2):
            pbs = []
            for b in range(3):
                wv, wn = wload(wo_views[b][:, :, half * 512:(half + 1) * 512], (4, 512))
                bkk = 1 + b
                for cc in range(4):
                    for kc in range(4):
                        P.op(PE, (lambda b, cc, kc, wv, bkk: lambda e: e.matmul(
                            bk(bkk)[:, cc * 128:(cc + 1) * 128], lhsT=wv[:, kc, cc * 128:(cc + 1) * 128],
                            rhs=brT[b][0][:, kc, :], start=(kc == 0), stop=(kc == 3)))(b, cc, kc, wv, bkk),
                            [wn, brT[b][1]], ["bank%d" % bkk])
            gl = lambda b: gatesT[:, b * 8 + half * 4:b * 8 + half * 4 + 4, :].rearrange("p c t -> p (c t)")
            gres = lambda b: ["gatesT%d" % (b * 8 + half * 4 + j) for j in range(4)]
            g0, g1, g2 = gl(0), gl(1), gl(2)
            P.op(DVE, (lambda g0: lambda e: e.tensor_tensor(out=m_acc[:], in0=bk(1)[:, :], in1=g0, op=ALU.mult))(g0),
                 ["bank1"] + gres(0), ["m_acc"])
            P.op(DVE, (lambda g1: lambda e: e.tensor_tensor(out=m_tmp[:], in0=bk(2)[:, :], in1=g1, op=ALU.mult))(g1),
                 ["bank2"] + gres(1), ["m_tmp"])
            P.op(DVE, lambda e: e.tensor_tensor(out=m_acc[:], in0=m_acc[:], in1=m_tmp[:], op=ALU.add),
                 ["m_acc", "m_tmp"], ["m_acc"])
            P.op(DVE, (lambda g2: lambda e: e.tensor_tensor(out=m_tmp[:], in0=bk(3)[:, :], in1=g2, op=ALU.mult))(g2),
                 ["bank3"] + gres(2), ["m_tmp"])
            P.op(DVE, (lambda half: lambda e: e.tensor_tensor(
                out=mT[:, half * 4:(half + 1) * 4, :].rearrange("p c t -> p (c t)"), in0=m_acc[:], in1=m_tmp[:],
                op=ALU.add))(half), ["m_acc", "m_tmp"], ["mT"])
        for half in range(2):
            wv, wn = wload(w_out_v[:, :, half * 512:(half + 1) * 512], (8, 512))
            bkk = 4 + half
            for kc in range(8):
                P.op(PE, (lambda kc, wv, bkk: lambda e: e.matmul(bk(bkk)[:, :], lhsT=mT[:, kc, :], rhs=wv[:, kc, :],
                                                                 start=(kc == 0), stop=(kc == 7)))(kc, wv, bkk),
                     ["mT", wn], ["bank%d" % bkk])
            P.op(DVE, (lambda half, bkk: lambda e: e.tensor_tensor(out=h_t[:, half * 512:(half + 1) * 512],
                                                                   in0=x_t[:, half * 512:(half + 1) * 512],
                                                                   in1=bk(bkk)[:, :], op=ALU.add))(half, bkk),
                 ["x_t", "bank%d" % bkk], ["x_t"])

    xn_b = n_b; xnT = nT
    qpT = sb([128, 16, 128], BF16, "qpT")
    k1T = sb([128, 128], BF16, "k1T"); k2T = sb([128, 128], BF16, "k2T")
    v16 = sb([128, 16, 16], F32, "v16"); i16 = sb([128, 16, 16], U32, "i16"); i16f = sb([128, 16, 16], F32, "i16f")
    work = sb([128, 128], F32, "work"); cand = sb([128, 8, 256], F32, "cand"); work2 = sb([128, 256], F32, "work2")
    sc16 = sb([128, 8, 16], F32, "sc16"); pos = sb([128, 8, 16], U32, "pos")
    aj_u = sb([128, 8, 16], U32, "aj_u"); bj_u = sb([128, 8, 16], U32, "bj_u")
    aj_f = sb([128, 8, 16], F32, "aj_f"); bj_f = sb([128, 8, 16], F32, "bj_f")
    oh = sb([128, 8, 16, 16], BF16, "oh")
    i1s = sb([128, 8, 16], F32, "i1s"); i2s = sb([128, 8, 16], F32, "i2s")
    eidx_f = sb([128, 128], F32, "eidx_f"); eidx_u = sb([128, 128], U32, "eidx_u")
    e16 = sb([128, 8, 16], F32, "e16"); zs = sb([128, 8], F32, "zs"); g16 = sb([128, 128], F32, "g16")
    a_t = sb([128, 128], F32, "a_t"); a2 = sb([128, 128], F32, "a2"); wgt = sb([128, 128], F32, "wgt")
    NSLOT = 8
    Ug = [sb([128, D], BF16, "Ug%d" % i) for i in range(NSLOT)]
    Vg = Ug
    w_pq_v = w_pq.rearrange("(c p) n -> p c n", p=128)

    def peer_setup():
        for src, dst, nm in ((pk1, k1T, "k1T"), (pk2, k2T, "k2T")):
            P.dma(SP, (lambda src: lambda e: e.dma_start(out=work[:], in_=src[:, :]))(src), [], ["work"])
            P.op(PE, lambda e: e.transpose(out=bk(0)[:, 0:128], in_=work[:], identity=ident_f), ["work", "cst_f"], ["bank0"])
            P.op(ACT, (lambda dst: lambda e: e.activation(out=dst[:], in_=bk(0)[:, 0:128], func=AF.Copy))(dst),
                 ["bank0"], [nm])

    def peer(y_out):
        rmsnorm_rows(h_t[:], "x_t", gffn_bc, "gffn_bc", xn_b[:], "n_b")
        transpose_rows(xn_b, "n_b", xnT, "nT")
        for blk in range(4):
            def ev_q(ps, res, blk=blk):
                P.op(ACT, lambda e: e.activation(out=qpT[:, blk * 4:(blk + 1) * 4, :].rearrange("p c t -> p (c t)"),
                                                 in_=ps, func=AF.Copy), [res], ["qpT"])
            proj_fm(w_pq_v[:, :, blk * 512:(blk + 1) * 512], 512, ev_q, xnT, "nT")
        for c in range(16):
            bkk = 4 + c // 4
            kT_ = k1T if c % 2 == 0 else k2T
            P.op(PE, (lambda c, bkk, kT_: lambda e: e.matmul(bk(bkk)[:, (c % 4) * 128:(c % 4 + 1) * 128], lhsT=qpT[:, c, :],
                                                             rhs=kT_[:], start=True, stop=True))(c, bkk, kT_),
                 ["qpT", "k1T", "k2T"], ["bank%d" % bkk])
        scv = lambda c: bk(4 + c // 4)[:, (c % 4) * 128:(c % 4 + 1) * 128]
        scr = lambda c: "bank%d" % (4 + c // 4)
        for c in range(16):
            P.op(DVE, (lambda c: lambda e: e.max(out=v16[:, c, 0:8], in_=scv(c)))(c), [scr(c)], ["v16"])
            P.op(DVE, (lambda c: lambda e: e.max_index(out=i16[:, c, 0:8], in_max=v16[:, c, 0:8], in_values=scv(c)))(c),
                 [scr(c), "v16"], ["i16"])
            P.op(DVE, (lambda c: lambda e: e.match_replace(out=work[:], in_to_replace=v16[:, c, 0:8], in_values=scv(c),
                                                           imm_value=NEG))(c), [scr(c), "v16"], ["work"])
            P.op(DVE, (lambda c: lambda e: e.max(out=v16[:, c, 8:16], in_=work[:]))(c), ["work"], ["v16"])
            P.op(DVE, (lambda c: lambda e: e.max_index(out=i16[:, c, 8:16], in_max=v16[:, c, 8:16], in_values=work[:]))(c),
                 ["work", "v16"], ["i16"])
        fl3 = lambda t: t[:].rearrange("p a b -> p (a b)")
        P.op(DVE, lambda e: e.tensor_copy(out=fl3(i16f), in_=fl3(i16)), ["i16"], ["i16f"])
        P.op(DVE, lambda e: e.tensor_tensor(out=cand[:].rearrange("p h (a b) -> p h a b", a=16),
                                            in0=AP(v16, 0, [[32, 8], [1, 16], [0, 16]]),
                                            in1=AP(v16, 16, [[32, 8], [0, 16], [1, 16]]), op=ALU.add), ["v16"], ["cand"])
        for h in range(8):
            P.op(DVE, (lambda h: lambda e: e.max(out=sc16[:, h, 0:8], in_=cand[:, h, :]))(h), ["cand"], ["sc16"])
            P.op(DVE, (lambda h: lambda e: e.max_index(out=pos[:, h, 0:8], in_max=sc16[:, h, 0:8], in_values=cand[:, h, :]))(h),
                 ["cand", "sc16"], ["pos"])
            P.op(DVE, (lambda h: lambda e: e.match_replace(out=work2[:], in_to_replace=sc16[:, h, 0:8],
                                                           in_values=cand[:, h, :], imm_value=NEG))(h),
                 ["cand", "sc16"], ["work2"])
            P.op(DVE, (lambda h: lambda e: e.max(out=sc16[:, h, 8:16], in_=work2[:]))(h), ["work2"], ["sc16"])
            P.op(DVE, (lambda h: lambda e: e.max_index(out=pos[:, h, 8:16], in_max=sc16[:, h, 8:16], in_values=work2[:]))(h),
                 ["work2", "sc16"], ["pos"])
        P.op(DVE, lambda e: e.tensor_single_scalar(out=fl3(aj_u), in_=fl3(pos), scalar=4, op=ALU.logical_shift_right),
             ["pos"], ["aj_u"])
        P.op(DVE, lambda e: e.tensor_single_scalar(out=fl3(bj_u), in_=fl3(pos), scalar=15, op=ALU.bitwise_and),
             ["pos"], ["bj_u"])
        P.op(DVE, lambda e: e.tensor_copy(out=fl3(aj_f), in_=fl3(aj_u)), ["aj_u"], ["aj_f"])
        P.op(DVE, lambda e: e.tensor_copy(out=fl3(bj_f), in_=fl3(bj_u)), ["bj_u"], ["bj_f"])
        for sel_f, sres, off, dst, dres in ((aj_f, "aj_f", 0, i1s, "i1s"), (bj_f, "bj_f", 16, i2s, "i2s")):
            P.op(DVE, (lambda sel_f: lambda e: e.tensor_tensor(out=oh[:], in0=AP(cst_f, 512, [[0, 8], [0, 16], [1, 16]]),
                                                               in1=AP(sel_f, 0, [[16, 8], [1, 16], [0, 16]]),
                                                               op=ALU.is_equal))(sel_f), ["cst_f", sres], ["oh"])
            P.op(DVE, (lambda off: lambda e: e.tensor_tensor(out=oh[:], in0=oh[:], in1=AP(i16f, off, [[32, 8], [0, 16], [1, 16]]),
                                                             op=ALU.mult))(off), ["oh", "i16f"], ["oh"])
            P.op(DVE, (lambda dst: lambda e: e.tensor_reduce(out=dst[:], in_=oh[:], axis=AX.X, op=ALU.add))(dst),
                 ["oh"], [dres])
        P.op(DVE, lambda e: e.scalar_tensor_tensor(out=eidx_f[:], in0=fl3(i1s), scalar=128.0, in1=fl3(i2s),
                                                   op0=ALU.mult, op1=ALU.add), ["i1s", "i2s"], ["eidx_f"])
        P.op(DVE, lambda e: e.tensor_scalar(out=eidx_f[:], in0=eidx_f[:], scalar1=0.0, scalar2=16383.0,
                                            op0=ALU.max, op1=ALU.min), ["eidx_f"], ["eidx_f"])
        P.op(DVE, lambda e: e.tensor_copy(out=eidx_u[:], in_=eidx_f[:]), ["eidx_f"], ["eidx_u"])
        P.op(DVE, lambda e: e.tensor_tensor(out=e16[:], in0=sc16[:], in1=AP(sc16, 0, [[16, 8], [0, 16]]), op=ALU.subtract),
             ["sc16"], ["e16"])
        P.op(ACT, lambda e: e.activation(out=fl3(e16), in_=fl3(e16), func=AF.Exp), ["e16"], ["e16"])
        P.op(DVE, lambda e: e.tensor_reduce(out=zs[:], in_=e16[:], axis=AX.X, op=ALU.add), ["e16"], ["zs"])
        P.op(DVE, lambda e: e.reciprocal(out=zs[:], in_=zs[:]), ["zs"], ["zs"])
        P.op(DVE, lambda e: e.tensor_tensor(out=g16[:].rearrange("p (h k) -> p h k", h=8), in0=e16[:],
                                            in1=AP(zs, 0, [[1, 8], [0, 16]]), op=ALU.mult), ["e16", "zs"], ["g16"])
        for hj in range(128):
            s = hj % NSLOT
            P.dma(POOL, (lambda hj, s: lambda e: e.indirect_dma_start(
                out=Ug[s][:], out_offset=None, in_=pu[:, :],
                in_offset=bass.IndirectOffsetOnAxis(ap=eidx_u[:, hj:hj + 1], axis=0)))(hj, s), ["eidx_u"], ["Ug%d" % s])
            P.op(DVE, (lambda hj, s: lambda e: e.scalar_tensor_tensor(
                out=junk_b[:], in0=Ug[s][:], scalar=1.0, in1=xn_b[:], op0=ALU.mult, op1=ALU.mult,
                accum_out=a_t[:, hj:hj + 1]))(hj, s), ["Ug%d" % s, "n_b"], ["junk_b", "a_t"])
        P.op(DVE, lambda e: e.tensor_tensor(out=a2[:], in0=a_t[:], in1=a_t[:], op=ALU.mult), ["a_t"], ["a2"])
        P.op(DVE, lambda e: e.tensor_scalar(out=a2[:], in0=a2[:], scalar1=0.044715, scalar2=1.0, op0=ALU.mult, op1=ALU.add),
             ["a2"], ["a2"])
        P.op(DVE, lambda e: e.tensor_tensor(out=a2[:], in0=a2[:], in1=a_t[:], op=ALU.mult), ["a2", "a_t"], ["a2"])
        P.op(ACT, lambda e: e.activation(out=a2[:], in_=a2[:], func=AF.Sigmoid, scale=1.5957691216057308), ["a2"], ["a2"])
        P.op(DVE, lambda e: e.tensor_tensor(out=a2[:], in0=a2[:], in1=a_t[:], op=ALU.mult), ["a2", "a_t"], ["a2"])
        P.op(DVE, lambda e: e.tensor_tensor(out=wgt[:], in0=a2[:], in1=g16[:], op=ALU.mult), ["a2", "g16"], ["wgt"])
        P.op(DVE, lambda e: e.memset(acc[:], 0.0), [], ["acc"])
        for hj in range(128):
            s = hj % NSLOT
            P.dma(POOL, (lambda hj, s: lambda e: e.indirect_dma_start(
                out=Vg[s][:], out_offset=None, in_=pv[:, :],
                in_offset=bass.IndirectOffsetOnAxis(ap=eidx_u[:, hj:hj + 1], axis=0)))(hj, s), ["eidx_u"], ["Ug%d" % s])
            P.op(DVE, (lambda hj, s: lambda e: e.scalar_tensor_tensor(out=acc[:], in0=Vg[s][:], scalar=wgt[:, hj:hj + 1],
                                                                      in1=acc[:], op0=ALU.mult, op1=ALU.add))(hj, s),
                 ["Ug%d" % s, "wgt", "acc"], ["acc"])
        P.op(DVE, lambda e: e.tensor_tensor(out=acc[:], in0=acc[:], in1=h_t[:], op=ALU.add), ["acc", "x_t"], ["acc"])
        rmsnorm_rows(acc[:], "acc", gfin_bc, "gfin_bc", acc[:], "acc")
        P.dma(SP, lambda e: e.dma_start(out=y_out, in_=acc[:]), ["acc"], [ores()])

    IOA = bass.IndirectOffsetOnAxis

    def sample_tile():
        Qbd = sb([128, 16, 4, 16], BF16, "Qbd")
        idx_u = sb([128, 256], U32, "idx_u"); pt_b = sb([128, 256], I32, "pt_b"); ptf = sb([128, 256], F32, "ptf")
        pt_pair = sb([128, 2], I32, "pt_pair")
        tot = sb([128, 2, 8], F32, "tot"); base = sb([128, 2, 8], F32, "base")
        nb_bias = sb([128, 8], F32, "nb_bias")
        PTs = sb([128, 1024], BF16, "PTs")
        Kpg = [sb([128, 512], BF16, "Kpg%d" % i) for i in range(2)]
        Vraw = [sb([128, 512], BF16, "Vraw%d" % i) for i in range(2)]
        o_un = sb([8, 528], F32, "o_un")
        lfP = VaugR[:].bitcast(F32)[:, 0:2048].rearrange("p (a n) -> p a n", a=2)
        Pb = oh[:].rearrange("p a b c -> p (a b c)").bitcast(F32).rearrange("p (h j) -> p h j", h=8)
        bias_s = cand[:].rearrange("p a b -> p (a b)").rearrange("p (n h) -> p n h", h=8)
        sc_s = acc
        Sfb = [m_acc, m_tmp]
        Sfr = ["m_acc", "m_tmp"]

        P.dma(SP, lambda e: e.dma_start(out=pt_b[:], in_=bc_rows(pt, 256)), [], ["pt_b"])
        P.dma(SP, lambda e: e.dma_start(out=pt_pair[:], in_=pt.rearrange("o (a p) -> p (o a)", p=128),
                                        allow_slow_non_contiguous=True), [], ["pt_pair"])
        P.op(DVE, lambda e: e.tensor_copy(out=ptf[:], in_=pt_b[:]), ["pt_b"], ["ptf"])
        P.op(DVE, lambda e: e.tensor_scalar(out=ptf[:], in0=ptf[:], scalar1=128.0, scalar2=cst_f[:, 544:545],
                                            op0=ALU.mult, op1=ALU.add), ["ptf", "cst_f"], ["ptf"])
        P.op(DVE, lambda e: e.tensor_copy(out=idx_u[:], in_=ptf[:]), ["ptf"], ["idx_u"])

        front(xs[:, :], 0, True)

        sg_k = sg.rearrange("n k v -> k n v")
        gss_k = gss.rearrange("n k v -> k n v")
        states = []
        for b in range(16):
            slot = (b // 2) % NSLOT
            bl = b % 2

            def pre_o(b=b, slot=slot):
                if b % 2 == 0:
                    dst = Ug[slot][:, :].rearrange("p (n v) -> p n v", n=8)
                    P.dma(POOL, lambda e: e.dma_start(out=dst, in_=sg_k[:, b * 4:b * 4 + 8, :]), [], ["Ug%d" % slot])

            def pre_s(b=b):
                dst = Sfb[b % 2][:, :].rearrange("p (n v) -> p n v", n=4)
                P.dma(SP, lambda e: e.dma_start(out=dst, in_=sg_k[:, b * 4:b * 4 + 4, :]), [], [Sfr[b % 2]])

            def post_s(b=b):
                src = Sfb[b % 2][:, :].rearrange("p (n v) -> p n v", n=4)
                P.dma(SP, lambda e: e.dma_start(out=gss_k[:, b * 4:b * 4 + 4, :], in_=src), [Sfr[b % 2]], [ores()])

            states.append(dict(
                Sf=(lambda h, b=b: Sfb[b % 2][:, h * 128:(h + 1) * 128]), rf=Sfr[b % 2],
                Sb=(lambda h, slot=slot, bl=bl: Ug[slot][:, (bl * 4 + h) * 128:(bl * 4 + h + 1) * 128]),
                rb="Ug%d" % slot, use_mask=True, pre_o=pre_o, pre_s=pre_s, post_s=post_s))
        if stage >= 2:
            gla(8, nbtri16, "nbtri16", btri_b, "btri_b", states)

        if stage >= 2:
            clf_pg = clf.rearrange("(n j) h -> n (j h)", j=128)
            for half in range(2):
                P.dma(POOL, (lambda half: lambda e: e.indirect_dma_start(
                    out=lfP[:, half, :], out_offset=None, in_=clf_pg[:, :],
                    in_offset=IOA(ap=pt_pair[:, half:half + 1].bitcast(U32), axis=0)))(half), ["pt_pair"], VAUG_ALL)
            for half in range(2):
                lv = lfP[:, half, :].rearrange("p (j h) -> p h j", h=8)
                P.op(DVE, (lambda half, lv: lambda e: e.tensor_reduce(out=tot[:, half, :], in_=lv, axis=AX.X, op=ALU.add))(half, lv),
                     VAUG_ALL, ["tot"])
                b1 = nbank()
                P.op(PE, (lambda half, b1: lambda e: e.matmul(bk(b1)[:, 0:8], lhsT=Mst_f, rhs=tot[:, half, :],
                                                              start=True, stop=False))(half, b1),
                     ["cst2_f", "tot"], ["bank%d" % b1])
                P.op(PE, (lambda half, b1: lambda e: e.matmul(bk(b1)[:, 0:8], lhsT=Bsel_f[half], rhs=lf_t[:],
                                                              start=False, stop=True))(half, b1),
                     ["cst2_f", "lf_t"], ["bank%d" % b1])
                P.op(DVE, (lambda half, b1: lambda e: e.tensor_tensor(out=base[:, half, :], in0=tot[:, half, :],
                                                                      in1=bk(b1)[:, 0:8], op=ALU.add))(half, b1),
                     ["tot", "bank%d" % b1], ["base"])
                for h in range(8):
                    P.op(DVE, (lambda h, lv: lambda e: e.tensor_tensor_scan(out=Pb[:, h, :], data0=ones_f[:, :], data1=lv[:, h, :],
                                                                            initial=0.0, op0=ALU.mult, op1=ALU.add))(h, lv),
                         VAUG_ALL + ["ones_f"], ["oh"])
                P.op(DVE, (lambda half: lambda e: e.tensor_tensor(out=Pb, in0=AP(base, half * 8, [[1, 8], [0, 128]]), in1=Pb,
                                                                  op=ALU.subtract))(half), ["base", "oh"], ["oh"])
                for h in range(8):
                    bkk = 4 + h // 4
                    P.op(PE, (lambda h, bkk: lambda e: e.transpose(out=bk(bkk)[:, (h % 4) * 128:(h % 4 + 1) * 128],
                                                                   in_=Pb[:, h, :], identity=ident_f))(h, bkk),
                         ["oh", "cst_f"], ["bank%d" % bkk])
                for q in range(2):
                    P.op(ACT, (lambda q, half: lambda e: e.activation(
                        out=bias_s[:, half * 128:(half + 1) * 128, q * 4:(q + 1) * 4],
                        in_=bk(4 + q)[:, :].rearrange("p (h t) -> p t h", h=4), func=AF.Copy))(q, half),
                        ["bank%d" % (4 + q)], ["cand"])
            for q in range(2):
                P.op(DVE, (lambda q: lambda e: e.memset(VaugR[:, q * 2112:(q + 1) * 2112], 1.0))(q), [], VAUG_ALL)
            b1 = nbank()
            P.op(PE, lambda e: e.matmul(bk(b1)[:, 0:8], lhsT=bustr8_f, rhs=lf_t[:], start=True, stop=True),
                 ["cst2_f", "lf_t"], ["bank%d" % b1])
            P.op(DVE, lambda e: e.tensor_copy(out=nb_bias[:], in_=bk(b1)[:, 0:8]), ["bank%d" % b1], ["nb_bias"])
            P.op(DVE, lambda e: e.memset(Qbd[:].rearrange("p b c q -> p (b c q)"), 0.0), [], ["Qbd"])
            for c in range(4):
                P.op(DVE, (lambda c: lambda e: e.tensor_copy(out=Qbd[0:64, :, c, 0:8],
                                                             in_=qfT[0:64, c, :].rearrange("p (b q) -> p b q", q=8)))(c),
                     ["qfT"], ["Qbd"])
                P.op(DVE, (lambda c: lambda e: e.tensor_copy(out=Qbd[64:128, :, c, 8:16],
                                                             in_=qfT[64:128, c, :].rearrange("p (b q) -> p b q", q=8)))(c),
                     ["qfT"], ["Qbd"])
            build_qbd()
            fox_block(lambda c: KTn[:, c, :], "KTn", lambda h: Vn[:, h, :], "Vn", nb_bias, "nb_bias",
                      btri_b, "btri_b", True, False)
            kk = 0
            pb0 = bk_bf(0)
            for b in range(16):
                for g in range(16):
                    pair = b * 16 + g
                    k = kk % 2
                    kk += 1
                    P.dma(POOL, (lambda k, pair: lambda e: e.indirect_dma_start(
                        out=Kpg[k][:], out_offset=None, in_=ck[:, :],
                        in_offset=IOA(ap=idx_u[:, pair:pair + 1], axis=0)))(k, pair), ["idx_u"], ["Kpg%d" % k])
                    P.dma(POOL, (lambda k, pair: lambda e: e.indirect_dma_start(
                        out=Vraw[k][:], out_offset=None, in_=cv[:, :],
                        in_offset=IOA(ap=idx_u[:, pair:pair + 1], axis=0)))(k, pair), ["idx_u"], ["Vraw%d" % k])
                    for c in range(4):
                        P.op(PE, (lambda k, c: lambda e: e.transpose(out=pb0[:, k * 512 + c * 128:k * 512 + (c + 1) * 128],
                                                                     in_=Kpg[k][:, c * 128:(c + 1) * 128], identity=ident_b[:]))(k, c),
                             ["Kpg%d" % k, "ident_b"], ["bank0"])
                    P.op(ACT, (lambda k, g: lambda e: e.activation(
                        out=KT[:, :, g * 128:(g + 1) * 128],
                        in_=pb0[:, k * 512:(k + 1) * 512].rearrange("p (c t) -> p c t", c=4), func=AF.Copy))(k, g),
                        ["bank0"], ["KT%d" % g])
                    P.op(DVE, (lambda k, g: lambda e: e.tensor_copy(out=Vaug[:, g, :, 0:64],
                                                                    in_=Vraw[k][:, :].rearrange("p (h d) -> p h d", h=8)))(k, g),
                         ["Vraw%d" % k], ["Vaug%d" % g])
                    for c in range(4):
                        bkk = 4 + g // 8
                        P.op(PE, (lambda g, c, b, bkk: lambda e: e.matmul(
                            bk(bkk)[:, (g % 8) * 64 + c * 16:(g % 8) * 64 + (c + 1) * 16], lhsT=KT[:, c, g * 128:(g + 1) * 128],
                            rhs=Qbd[:, b, c, :], start=True, stop=True))(g, c, b, bkk),
                            ["KT%d" % g, "Qbd"], ["bank%d" % bkk])
                for gh in range(2):
                    P.op(DVE, (lambda gh, b: lambda e: e.scalar_tensor_tensor(
                        out=sc_s[:, gh * 512:(gh + 1) * 512].rearrange("p (n q) -> p n q", q=8),
                        in0=bk(4 + gh)[:, :].rearrange("p (n q) -> p n q", q=8), scalar=0.125,
                        in1=AP(cand, b * 128 + gh * 64, [[1, 64], [0, 8]]), op0=ALU.mult, op1=ALU.add))(gh, b),
                        ["bank%d" % (4 + gh), "cand"], ["acc"])
                P.op(ACT, lambda e: e.activation(out=PTs[:], in_=sc_s[:], func=AF.Exp), ["acc"], ["PTs"])
                for g in range(16):
                    for h in range(8):
                        bkk = 1 + h // 4
                        P.op(PE, (lambda g, h, bkk: lambda e: e.matmul(
                            bk(bkk)[0:8, (h % 4) * 66:(h % 4) * 66 + 66], lhsT=PTs[:, g * 64 + h * 8:g * 64 + h * 8 + 8],
                            rhs=Vaug[:, g, h, :], start=(g == 0 and h % 4 == 0), stop=(g == 15),
                            skip_group_check=True))(g, h, bkk), ["PTs", "Vaug%d" % g], ["bank%d" % bkk])
                for q in range(2):
                    P.op(ACT, (lambda q: lambda e: e.activation(out=o_un[0:8, q * 264:(q + 1) * 264], in_=bk(1 + q)[0:8, 0:264],
                                                                func=AF.Copy))(q), ["bank%d" % (1 + q)], ["o_un"])
                for q in range(2):
                    P.op(PE, (lambda q, b: lambda e: e.matmul(
                        bk(6 + q)[:, 0:264], lhsT=Esel_f[0:8, 120 - 8 * b:248 - 8 * b], rhs=o_un[0:8, q * 264:(q + 1) * 264],
                        start=False, stop=(b == 15), skip_group_check=True))(q, b), ["cst2_f", "o_un"], ["bank%d" % (6 + q)])
            attn_finish(8, 64, ofT, "ofT")

        if stage >= 3:
            for k in range(2):
                P.op(DVE, (lambda k: lambda e: e.memset(PTm[k][:].rearrange("p a t -> p (a t)"), 0.0))(k), [], ["PT%d" % k])
            pb0 = bk_bf(0)
            for b in range(16):
                k = b % 2
                mkb, mkn = wload(cmk[b].rearrange("(mb p) n -> p mb n", p=128), (2, 512))
                mvb, mvn = wload(cmv[b].rearrange("(mb p) n -> p mb n", p=128), (2, 512))
                for mb in range(2):
                    for h in range(4):
                        P.op(PE, (lambda mb, h, mkb: lambda e: e.transpose(
                            out=pb0[:, (mb * 4 + h) * 128:(mb * 4 + h + 1) * 128], in_=mkb[:, mb, h * 128:(h + 1) * 128],
                            identity=ident_b[:]))(mb, h, mkb), [mkn, "ident_b"], ["bank0"])
                    P.op(ACT, (lambda mb: lambda e: e.activation(
                        out=mkT[:, :, mb * 128:(mb + 1) * 128],
                        in_=pb0[:, mb * 512:(mb + 1) * 512].rearrange("p (c t) -> p c t", c=4), func=AF.Copy))(mb),
                        ["bank0"], ["mkT"])
                P.op(DVE, (lambda mvb: lambda e: e.tensor_copy(out=mvA[:, :, :, 0:128],
                                                               in_=mvb.rearrange("p mb (h d) -> p mb h d", h=4)))(mvb),
                     [mvn], ["mvA"])
                mem_attend_block(lambda h, mb: mkT[:, h, mb * 128:(mb + 1) * 128], "mkT",
                                 lambda h, mb: mvA[:, mb, h, :], "mvA", PTm[k], "PT%d" % k, None, (b * 8, 8), b == 0, b == 15)
                P.op(DVE, (lambda k, b: lambda e: e.memset(PTm[k][:, :, b * 8:(b + 1) * 8], 0.0))(k, b), [], ["PT%d" % k])
            attn_finish(4, 128, omT, "omT")
        if stage >= 4:
            merge_out()
        if stage >= 5:
            peer(ys[:, :])

    prompt_state = [dict(Sf=lambda h: S_f[:, h, :], rf="S_f", Sb=lambda h: S_b[:, h, :], rb="S_b", use_mask=False)]
    if stage >= 3:
        mem_setup_prompt()
    if stage >= 5:
        peer_setup()
    for ti in range(ntiles):
        front(xp[ti * 128:(ti + 1) * 128, :], ti, False)
        if stage >= 2:
            gla(128, ntri16, "ntri16", tri_b, "tri_b", prompt_state)
            P.op(ACT, lambda e: e.activation(out=S_b[:].rearrange("p h v -> p (h v)"),
                                             in_=S_f[:].rearrange("p h v -> p (h v)"), func=AF.Copy), ["S_f"], ["S_b"])
            fox_prompt(ti)
        if stage >= 3:
            k = ti % 2
            mem_attend_block(lambda h, mb: mkT[:, h, mb * 128:(mb + 1) * 128], "mkT",
                             lambda h, mb: mvA[:, mb, h, :], "mvA", PTm[k], "PT%d" % k, None, (0, 128), True, True)
            attn_finish(4, 128, omT, "omT")
        if stage >= 4:
            merge_out()
        if stage >= 5:
            peer(yp[ti * 128:(ti + 1) * 128, :])
    if stage >= 2:
        P.dma(SP, lambda e: e.dma_start(out=gsp.rearrange("h k v -> k h v"), in_=S_f[:]), ["S_f"], [ores()])
    if sample:
        sample_tile()

    if maxops is not None:
        for i, o in enumerate(P.ops[:maxops]):
            print(i, o.eng, "dma" if o.dma else "", o.reads, "->", o.writes)
        P.ops = P.ops[:maxops]
    P.op(SP, None, list(out_res), [])
    P.finalize(es)
    print("ops", len(P.ops), "max_sem", P.max_sem)
    return nc, es


def make_consts2():
    c = np.zeros((128, 760), np.float32)
    a = np.arange(128)[:, None]; b = np.arange(128)[None, :]
    c[:, 0:128] = ((a // 16) == (b // 16)) & (a > b)
    c[:, 128:256] = ((a // 8) == (b // 16))
    c[:, 256:384] = ((a // 8) == (8 + b // 16))
    c[:, 384:512] = ((a // 8) == (b // 8)) & (a > b)
    cc = np.arange(248)[None, :]
    c[:, 512:760] = (cc == 120 + a) & (a < 8)
    return c


def make_consts():
    c = np.zeros((128, 560), np.float32)
    j = np.arange(128)[:, None]; t = np.arange(128)[None, :]
    c[:, 0:128] = (j == t)
    c[:, 128:256] = (j <= t)
    c[:, 256:384] = (j <= t) & ((j // 8) == (t // 8))
    c[:, 384:512] = (j > t)
    c[:, 512:528] = np.arange(16)[None, :]
    c[:, 528:544] = ((np.arange(128)[:, None] // 8) == np.arange(16)[None, :])
    c[:, 544] = np.arange(128)
    return c


def run(inp, stage=99, ntiles=NT, sample=1, small_pool=False, compact=False, maxops=None):
    f = lambda a: np.ascontiguousarray(np.asarray(a))
    nphys = 4 if small_pool else (256 if compact else NPHYS)
    nexp = 128 if stage < 5 else 16384
    nc, es = build(stage=stage, nphys=nphys, ntiles=ntiles, sample=sample, maxops=maxops, nexp=nexp)
    cst = make_consts()
    if small_pool:
        ck = np.zeros((nphys * 128, 512), np.float32); cv = ck; clf = np.zeros((nphys * 128, 8), np.float32)
    elif compact:
        ck = cv = clf = None
    else:
        ck = f(inp["cache_fox_k"]).reshape(NPHYS * 128, 512)
        cv = f(inp["cache_fox_v"]).reshape(NPHYS * 128, 512)
        clf = f(inp["cache_fox_logf"]).reshape(NPHYS * 128, 8)
    shared = {
        "ck": ck, "cv": cv, "clf": clf,
        "g_mix": f(inp["g_mix"]).reshape(1, D), "w_in": f(inp["w_in"])[0], "w_a2": f(inp["w_a2"])[0],
        "b_a2": f(inp["b_a2"]).reshape(1, 512), "b_fgate": f(inp["b_fgate"]).reshape(1, 8),
        "b_gate": f(inp["b_gate"]).reshape(1, 3072), "g_gh": f(inp["g_gla_head"]).reshape(1, 128),
        "w_gla_o": f(inp["w_gla_o"])[0], "w_fox_o": f(inp["w_fox_o"])[0], "w_mem_o": f(inp["w_mem_o"])[0],
        "w_out": f(inp["w_out"])[0], "g_mem": f(inp["g_mem"]).reshape(1, D), "w_mem_kv": f(inp["w_mem_kv"])[0],
        "g_ffn": f(inp["g_ffn"]).reshape(1, D), "w_pq": f(inp["w_pq"])[0], "pk1": f(inp["peer_k1"])[0],
        "pk2": f(inp["peer_k2"])[0], "pu": f(inp["peer_u"])[0][:nexp], "pv": f(inp["peer_v"])[0][:nexp],
        "g_final": f(inp["g_final"]).reshape(1, D), "cst": cst, "cst2": make_consts2(),
    }
    in_maps = []
    for c in range(NCORES):
        m = dict(shared)
        m["xp"] = f(inp["x_prompt"][c])
        m["xs"] = f(inp["x_sample"][16 * c:16 * c + 16]).reshape(128, D)
        m["memp"] = f(inp["mem_prompt"][c])
        m["sg"] = f(inp["state_gla"][0, 16 * c:16 * c + 16]).reshape(64, 128, 128)
        m["cmk"] = f(inp["cache_mem_k"][0, 16 * c:16 * c + 16]).reshape(16, 256, 512)
        m["cmv"] = f(inp["cache_mem_v"][0, 16 * c:16 * c + 16]).reshape(16, 256, 512)
        m["pt"] = f(inp["page_table"][16 * c:16 * c + 16]).reshape(1, 256).astype(np.int32)
        if compact:
            ptc = m["pt"].reshape(256)
            perm = np.random.RandomState(c).permutation(256)
            inv = np.empty(256, np.int64); inv[perm] = np.arange(256)
            m["ck"] = f(inp["cache_fox_k"][0][ptc[perm]]).reshape(256 * 128, 512)
            m["cv"] = f(inp["cache_fox_v"][0][ptc[perm]]).reshape(256 * 128, 512)
            m["clf"] = f(inp["cache_fox_logf"][0][ptc[perm]]).reshape(256 * 128, 8)
            m["pt"] = inv.astype(np.int32).reshape(1, 256)
        in_maps.append(m)
    res = run_bass_kernel_spmd(nc, in_maps, core_ids=list(range(NCORES)))
    es.close()
    R = res.results
    cat = lambda k: np.stack([R[c][k] for c in range(NCORES)])
    y_prompt = cat("yp")
    y_sample = cat("ys").reshape(128, 8, D)
    fkp = cat("fkp").reshape(1, 8, SEQ, 8, 64)
    fvp = cat("fvp").reshape(1, 8, SEQ, 8, 64)
    lfp = cat("lfp").reshape(1, 8, SEQ, 8)
    gsp = cat("gsp").reshape(1, 8, 4, 128, 128)
    mkp = cat("mkp").reshape(1, 8, 256, 4, 128)
    mvp = cat("mvp").reshape(1, 8, 256, 4, 128)
    fks = cat("fks").reshape(1, 128, 8, 8, 64)
    fvs = cat("fvs").reshape(1, 128, 8, 8, 64)
    lfs = cat("lfs").reshape(1, 128, 8, 8)
    gss = cat("gss").reshape(1, 128, 4, 128, 128)
    return (y_prompt, y_sample, fkp, fvp, lfp, gsp, mkp, mvp, fks, fvs, lfs, gss)


def kernel(**inp):
    return run(inp)
```
